# Optimizing a Trainium2 kernel written in Bass

```python
import jax, jax.numpy as jnp
from jax import lax
import numpy as np

D_MODEL = 1024
BATCH = 8
SEQ = 2048
DEPTH = 4

N_MEM = 256
MLP_HIDDEN = 4 * D_MODEL
RMS_EPS = 1e-6
LN_EPS = 1e-5
MASK_VALUE = -1e30
MIN_FORGET = 1e-20

N_MIXERS = 3
LAYER_KINDS = tuple(i % N_MIXERS for i in range(DEPTH))
N_PER_KIND = tuple(LAYER_KINDS.count(k) for k in range(N_MIXERS))

HGRN_HEAD_DIM = 128
HGRN_HEADS = D_MODEL // HGRN_HEAD_DIM
HGRN_WIDTH = HGRN_HEADS * HGRN_HEAD_DIM
HGRN_CHUNK = 64

SWA_HEAD_DIM = 64
SWA_Q_HEADS = D_MODEL // SWA_HEAD_DIM
SWA_KV_HEADS = 2
SWA_GROUP = SWA_Q_HEADS // SWA_KV_HEADS
SWA_WINDOW = 128
SWA_BLOCK = SWA_WINDOW
SWA_Q_WIDTH = SWA_Q_HEADS * SWA_HEAD_DIM
SWA_KV_WIDTH = SWA_KV_HEADS * SWA_HEAD_DIM
ROPE_THETA = 10000.0

CONV_DIM = D_MODEL
CONV_WIDTH = 31

XA_HEADS = 4
XA_HEAD_DIM = 128
XA_WIDTH = XA_HEADS * XA_HEAD_DIM

A_IN = 4 * HGRN_WIDTH + XA_WIDTH
B_IN = SWA_Q_WIDTH + 2 * SWA_KV_WIDTH + XA_WIDTH
C_IN = 2 * CONV_DIM + XA_WIDTH

kernel_name = "hybrid_hgrn2_swa_conformer_trunk"


def rms_norm(x, g):
    xf = x.astype(jnp.float32)
    y = xf * lax.rsqrt(jnp.mean(xf * xf, axis=-1, keepdims=True) + RMS_EPS)
    return (y * g.astype(jnp.float32)).astype(x.dtype)


def layer_norm(x, g, b):
    xf = x.astype(jnp.float32)
    mu = jnp.mean(xf, axis=-1, keepdims=True)
    var = jnp.mean(jnp.square(xf - mu), axis=-1, keepdims=True)
    y = (xf - mu) * lax.rsqrt(var + LN_EPS) * g.astype(jnp.float32) + b.astype(jnp.float32)
    return y.astype(x.dtype)


def rope_tables(positions):
    half = SWA_HEAD_DIM // 2
    inv_freq = ROPE_THETA ** (-jnp.arange(half, dtype=jnp.float32) / half)
    ang = positions.astype(jnp.float32)[..., None] * inv_freq
    return jnp.cos(ang)[:, :, None, :], jnp.sin(ang)[:, :, None, :]


def apply_rope(x, cos, sin):
    half = x.shape[-1] // 2
    xf = x.astype(jnp.float32)
    x1, x2 = xf[..., :half], xf[..., half:]
    return jnp.concatenate([x1 * cos - x2 * sin, x2 * cos + x1 * sin], axis=-1).astype(x.dtype)


def hgrn_lower_bounds(logits):
    p = jax.nn.softmax(logits.astype(jnp.float32), axis=0)
    return jnp.cumsum(p, axis=0) - p[0:1]


def hgrn2_chunked(q, k, v, log_f):
    B, H, T, dk = q.shape
    dv = v.shape[-1]
    C = HGRN_CHUNK
    N = T // C

    def to_chunks(a):
        return a.reshape(B, H, N, C, a.shape[-1]).transpose(2, 0, 1, 3, 4)

    causal = jnp.tril(jnp.ones((C, C), dtype=bool))[:, :, None]

    def step(S, inp):
        qc, kc, vc, gc = inp
        b = jnp.cumsum(gc, axis=2)
        diff = b[:, :, :, None, :] - b[:, :, None, :, :]
        decay = jnp.where(causal, jnp.exp(jnp.minimum(diff, 0.0)), 0.0)
        attn = jnp.einsum('bhik,bhijk,bhjk->bhij', qc, decay, kc)
        o = (jnp.einsum('bhij,bhjv->bhiv', attn, vc)
             + jnp.einsum('bhik,bhkv->bhiv', qc * jnp.exp(b), S))
        b_last = b[:, :, -1:, :]
        S = (jnp.exp(b_last[:, :, 0, :])[..., None] * S
             + jnp.einsum('bhjk,bhjv->bhkv', kc * jnp.exp(b_last - b), vc))
        return S, o

    S0 = jnp.zeros((B, H, dk, dv), jnp.float32)
    _, o = lax.scan(step, S0, (to_chunks(q), to_chunks(k), to_chunks(v), to_chunks(log_f)))
    return o.transpose(1, 2, 0, 3, 4).reshape(B, H, T, dv)


def hgrn2_mixer(proj, lb, o_norm_g):
    B, T, _ = proj.shape
    q, f_raw, inp, gate = jnp.split(proj, 4, axis=-1)

    def heads(a):
        return a.reshape(B, T, HGRN_HEADS, HGRN_HEAD_DIM).transpose(0, 2, 1, 3).astype(jnp.float32)

    lb_h = lb.astype(jnp.float32).reshape(1, HGRN_HEADS, 1, HGRN_HEAD_DIM)
    fr = heads(f_raw)
    f = lb_h + (1.0 - lb_h) * jax.nn.sigmoid(fr)
    log_f = jnp.log(jnp.maximum(f, MIN_FORGET))
    k = 1.0 - f
    o = hgrn2_chunked(heads(q), k, heads(inp), log_f)
    o = o.transpose(0, 2, 1, 3)
    o = rms_norm(o, o_norm_g.reshape(HGRN_HEADS, HGRN_HEAD_DIM))
    return o.reshape(B, T, HGRN_WIDTH).astype(proj.dtype) * jax.nn.silu(gate)


def swa_mixer(proj, cos, sin, q_g, k_g, sinks):
    B, T, _ = proj.shape
    hd, KV, G, BLK = SWA_HEAD_DIM, SWA_KV_HEADS, SWA_GROUP, SWA_BLOCK
    q = proj[..., :SWA_Q_WIDTH].reshape(B, T, SWA_Q_HEADS, hd)
    k = proj[..., SWA_Q_WIDTH:SWA_Q_WIDTH + SWA_KV_WIDTH].reshape(B, T, KV, hd)
    v = proj[..., SWA_Q_WIDTH + SWA_KV_WIDTH:].reshape(B, T, KV, hd)
    q = apply_rope(rms_norm(q, q_g), cos, sin)
    k = apply_rope(rms_norm(k, k_g), cos, sin)
    N = T // BLK
    qb = q.reshape(B, N, BLK, KV, G, hd)

    def with_prev_block(a):
        ab = jnp.pad(a, ((0, 0), (BLK, 0), (0, 0), (0, 0))).reshape(B, N + 1, BLK, KV, hd)
        return jnp.concatenate([ab[:, :-1], ab[:, 1:]], axis=2)

    kb, vb = with_prev_block(k), with_prev_block(v)
    scores = jnp.einsum('bnqkgd,bnskd->bnkgqs', qb, kb).astype(jnp.float32) * (hd ** -0.5)
    qi = jnp.arange(BLK)[:, None]
    si = jnp.arange(2 * BLK)[None, :]
    rel = qi + BLK - si
    in_window = (rel >= 0) & (rel < SWA_WINDOW)
    key_pos = jnp.arange(N)[:, None] * BLK - BLK + si
    valid = in_window[None] & (key_pos >= 0)[:, None, :]
    scores = jnp.where(valid[None, :, None, None], scores, MASK_VALUE)
    sink = sinks.astype(jnp.float32).reshape(KV, G)[None, None, :, :, None, None]
    m = jnp.maximum(jnp.max(scores, axis=-1, keepdims=True), sink)
    p = jnp.exp(scores - m)
    p = p / (jnp.sum(p, axis=-1, keepdims=True) + jnp.exp(sink - m))
    out = jnp.einsum('bnkgqs,bnskd->bnqkgd', p.astype(v.dtype), vb)
    return out.reshape(B, T, SWA_Q_WIDTH)


def conformer_conv_mixer(proj, conv_w, conv_b, ln_g, ln_b):
    a, gate = proj[..., :CONV_DIM], proj[..., CONV_DIM:]
    u = a * jax.nn.sigmoid(gate)
    y = lax.conv_general_dilated(
        u, conv_w[:, None, :].astype(u.dtype), window_strides=(1,),
        padding=[(CONV_WIDTH - 1, 0)], dimension_numbers=('NWC', 'WIO', 'NWC'),
        feature_group_count=CONV_DIM) + conv_b
    y = layer_norm(y, ln_g, ln_b)
    return jax.nn.silu(y)


def memory_cross_attention(xq, memn, w_kv, q_g, k_g):
    B, T, _ = xq.shape
    M = memn.shape[1]
    q = rms_norm(xq.reshape(B, T, XA_HEADS, XA_HEAD_DIM), q_g)
    kv = memn @ w_kv
    k = rms_norm(kv[..., :XA_WIDTH].reshape(B, M, XA_HEADS, XA_HEAD_DIM), k_g)
    v = kv[..., XA_WIDTH:].reshape(B, M, XA_HEADS, XA_HEAD_DIM)
    s = jnp.einsum('bthd,bmhd->bhtm', q, k).astype(jnp.float32) * (XA_HEAD_DIM ** -0.5)
    p = jax.nn.softmax(s, axis=-1).astype(v.dtype)
    return jnp.einsum('bhtm,bmhd->bthd', p, v).reshape(B, T, XA_WIDTH)


def squared_relu_mlp(h, w_up, w_down):
    return jnp.square(jax.nn.relu(h @ w_up)) @ w_down


def setup_inputs(seed: int = 0) -> dict:
    key = jax.random.key(seed)
    ks = iter(jax.random.split(key, 64))

    def nrm(shape, scale):
        return scale * jax.random.normal(next(ks), shape, jnp.float32)

    def gain(shape):
        return 1.0 + 0.05 * jax.random.normal(next(ks), shape, jnp.float32)

    NA, NB, NC = N_PER_KIND
    res = 0.5
    offsets = jax.random.randint(next(ks), (BATCH, 1), 0, 4096, dtype=jnp.int32)
    positions = offsets + jnp.arange(SEQ, dtype=jnp.int32)[None, :]
    return {
        "x": nrm((BATCH, SEQ, D_MODEL), 1.0),
        "mem": nrm((BATCH, N_MEM, D_MODEL), 1.0),
        "positions": positions,
        "norm_mix_g": gain((DEPTH, D_MODEL)),
        "norm_mlp_g": gain((DEPTH, D_MODEL)),
        "mem_norm_g": gain((DEPTH, D_MODEL)),
        "xa_w_kv": nrm((DEPTH, D_MODEL, 2 * XA_WIDTH), D_MODEL ** -0.5),
        "xa_q_norm_g": gain((DEPTH, XA_HEAD_DIM)),
        "xa_k_norm_g": gain((DEPTH, XA_HEAD_DIM)),
        "mlp_w_up": nrm((DEPTH, D_MODEL, MLP_HIDDEN), D_MODEL ** -0.5),
        "mlp_w_down": nrm((DEPTH, MLP_HIDDEN, D_MODEL), res * MLP_HIDDEN ** -0.5),
        "hgrn_lb_logits": nrm((DEPTH, HGRN_WIDTH), 0.5),
        "a_w_in": nrm((NA, D_MODEL, A_IN), D_MODEL ** -0.5),
        "a_o_norm_g": gain((NA, HGRN_WIDTH)),
        "a_w_out": nrm((NA, HGRN_WIDTH + XA_WIDTH, D_MODEL), res * (HGRN_WIDTH + XA_WIDTH) ** -0.5),
        "b_w_in": nrm((NB, D_MODEL, B_IN), D_MODEL ** -0.5),
        "b_q_norm_g": gain((NB, SWA_HEAD_DIM)),
        "b_k_norm_g": gain((NB, SWA_HEAD_DIM)),
        "b_sinks": nrm((NB, SWA_Q_HEADS), 0.5),
        "b_w_out": nrm((NB, SWA_Q_WIDTH + XA_WIDTH, D_MODEL), res * (SWA_Q_WIDTH + XA_WIDTH) ** -0.5),
        "c_w_in": nrm((NC, D_MODEL, C_IN), D_MODEL ** -0.5),
        "c_conv_w": nrm((NC, CONV_WIDTH, CONV_DIM), CONV_WIDTH ** -0.5),
        "c_conv_b": nrm((NC, CONV_DIM), 0.02),
        "c_ln_g": gain((NC, CONV_DIM)),
        "c_ln_b": nrm((NC, CONV_DIM), 0.02),
        "c_w_out": nrm((NC, CONV_DIM + XA_WIDTH, D_MODEL), res * (CONV_DIM + XA_WIDTH) ** -0.5),
    }


def reference(x, mem, positions, norm_mix_g, norm_mlp_g, mem_norm_g, xa_w_kv, xa_q_norm_g,
              xa_k_norm_g, mlp_w_up, mlp_w_down, hgrn_lb_logits, a_w_in, a_o_norm_g, a_w_out,
              b_w_in, b_q_norm_g, b_k_norm_g, b_sinks, b_w_out, c_w_in, c_conv_w, c_conv_b,
              c_ln_g, c_ln_b, c_w_out):
    lb_all = hgrn_lower_bounds(hgrn_lb_logits)
    cos, sin = rope_tables(positions)
    for layer in range(DEPTH):
        kind = LAYER_KINDS[layer]
        idx = LAYER_KINDS[:layer].count(kind)
        h = rms_norm(x, norm_mix_g[layer])
        memn = rms_norm(mem, mem_norm_g[layer])
        if kind == 0:
            proj = h @ a_w_in[idx]
            mix = hgrn2_mixer(proj[..., :-XA_WIDTH], lb_all[layer], a_o_norm_g[idx])
            w_out = a_w_out[idx]
        elif kind == 1:
            proj = h @ b_w_in[idx]
            mix = swa_mixer(proj[..., :-XA_WIDTH], cos, sin, b_q_norm_g[idx], b_k_norm_g[idx], b_sinks[idx])
            w_out = b_w_out[idx]
        else:
            proj = h @ c_w_in[idx]
            mix = conformer_conv_mixer(proj[..., :-XA_WIDTH], c_conv_w[idx], c_conv_b[idx], c_ln_g[idx], c_ln_b[idx])
            w_out = c_w_out[idx]
        xa = memory_cross_attention(proj[..., -XA_WIDTH:], memn, xa_w_kv[layer],
                                    xa_q_norm_g[layer], xa_k_norm_g[layer])
        x = x + jnp.concatenate([mix, xa], axis=-1) @ w_out
        h2 = rms_norm(x, norm_mlp_g[layer])
        x = x + squared_relu_mlp(h2, mlp_w_up[layer], mlp_w_down[layer])
    return x
```

```python
import contextlib
import math
import numpy as np
import concourse.bass as bass
import concourse.mybir as mybir
from concourse.bass_utils import run_bass_kernel_spmd

F32 = mybir.dt.float32
BF16 = mybir.dt.bfloat16
I32 = mybir.dt.int32
AF = mybir.ActivationFunctionType
ALU = mybir.AluOpType

ENGS = ("pe", "act", "dve", "pool", "sp")
D = 1024
T = 2048
NG = 4
GS = 512
DEPTH = 4
KINDS = (0, 1, 2, 0)
RMS_EPS = 1e-6
LN_EPS = 1e-5
TWO_PI = 2.0 * math.pi


class Trk:
    __slots__ = ("w", "r")

    def __init__(self):
        self.w = None
        self.r = []


def trks(*shape):
    if len(shape) == 1:
        return [Trk() for _ in range(shape[0])]
    return [trks(*shape[1:]) for _ in range(shape[0])]


class Prog:
    def __init__(self, nc, n_dma_sems=32):
        self.nc = nc
        self.es = contextlib.ExitStack()
        self.streams = {e: [] for e in ENGS}
        self.count = {e: 0 for e in ENGS}
        self.pending = {e: False for e in ENGS}
        self.waited = {e: {} for e in ENGS}
        self.sems = {}
        for e in ("pe", "act", "dve", "pool"):
            self.sems[e] = self.es.enter_context(nc.semaphore("s_" + e))
        self.dma_sems = []
        for i in range(n_dma_sems):
            k = "d%d" % i
            self.sems[k] = self.es.enter_context(nc.semaphore("s_" + k))
            self.dma_sems.append(k)
        self.dma_val = {k: 0 for k in self.dma_sems}
        self.dma_rr = 0
        self.ntens = 0
        self.NSW = 4
        self.sw_sems = [self.es.enter_context(nc.semaphore("s_sw%d" % i)) for i in range(self.NSW)]
        self.sems["rel"] = self.es.enter_context(nc.semaphore("s_rel"))
        self.sw_issued = 0
        self.sw_relayed = 0

    def sbuf(self, shape, dtype):
        self.ntens += 1
        return self.es.enter_context(self.nc.sbuf_tensor("t%d" % self.ntens, list(shape), dtype))

    def psum(self, shape, dtype):
        self.ntens += 1
        return self.es.enter_context(self.nc.psum_tensor("p%d" % self.ntens, list(shape), dtype))

    @staticmethod
    def _deps(reads, writes):
        deps = {}

        def add(d):
            if d is not None and deps.get(d[0], 0) < d[1]:
                deps[d[0]] = d[1]
        for t in reads:
            add(t.w)
        for t in writes:
            add(t.w)
            for d in t.r:
                add(d)
        return deps

    def _emit_waits(self, eng, deps):
        for k, v in deps.items():
            if k == eng:
                if eng == "pe" or v <= self.count[eng] - 10 or v > self.count[eng]:
                    continue
            if self.waited[eng].get(k, 0) >= v:
                continue
            self.waited[eng][k] = v
            sem = self.sems[k]
            self.streams[eng].append(lambda e, sem=sem, v=v: e.wait_ge(sem, v))

    def _mark(self, tok, reads, writes):
        for t in reads:
            t.r.append(tok)
            if len(t.r) > 64:
                m = {}
                for k, v in t.r:
                    if m.get(k, 0) < v:
                        m[k] = v
                t.r = list(m.items())
        for t in writes:
            t.w = tok
            t.r = []

    log = None

    def op(self, eng, fn, reads=(), writes=(), inc=True, cost=None):
        if self.log is not None:
            self.log.append((eng, cost))
        self._emit_waits(eng, self._deps(reads, writes))
        tok = (eng, self.count[eng] + 1)
        if inc:
            sem = self.sems[eng]
            self.streams[eng].append(lambda e, fn=fn, sem=sem: fn(e).then_inc(sem, 1))
            self.count[eng] += 1
            self.pending[eng] = False
        else:
            self.streams[eng].append(lambda e, fn=fn: fn(e))
            self.pending[eng] = True
        self._mark(tok, reads, writes)
        return tok

    def dma(self, q, out, in_, reads=(), writes=()):
        deps = self._deps(reads, writes)
        k = self.dma_sems[self.dma_rr % len(self.dma_sems)]
        self.dma_rr += 1
        if self.dma_val[k] > 0:
            deps[k] = max(deps.get(k, 0), self.dma_val[k])
        self._emit_waits(q, deps)
        self.dma_val[k] += 16
        tok = (k, self.dma_val[k])
        sem = self.sems[k]
        self.streams[q].append(lambda e, out=out, in_=in_, sem=sem: e.dma_start(out=out, in_=in_).then_inc(sem, 16))
        self._mark(tok, reads, writes)
        return tok

    def dma_sw(self, out, in_, reads=(), writes=()):
        i = self.sw_issued
        if i - self.sw_relayed >= self.NSW:
            self.relay_upto(i - self.NSW)
        self._emit_waits("pool", self._deps(reads, writes))
        sem = self.sw_sems[i % self.NSW]
        self.streams["pool"].append(lambda e, out=out, in_=in_, sem=sem: e.dma_start(out=out, in_=in_).then_inc(sem, 16))
        self.sw_issued += 1
        self._mark(("rel", i + 1), reads, writes)
        return i

    def relay_upto(self, i):
        rel = self.sems["rel"]
        while self.sw_relayed <= min(i, self.sw_issued - 1):
            sem = self.sw_sems[self.sw_relayed % self.NSW]

            def f(e, sem=sem, rel=rel):
                e.wait_ge(sem, 16)
                e.sem_inc(sem, -16)
                e.sem_inc(rel, 1)
            self.streams["pool"].append(f)
            self.sw_relayed += 1

    def wait_tok(self, eng, toks):
        deps = {}
        for k, v in toks:
            if deps.get(k, 0) < v:
                deps[k] = v
        self._emit_waits(eng, deps)

    def emit(self):
        nc = self.nc
        self.relay_upto(self.sw_issued - 1)
        for e in ENGS:
            assert not self.pending[e], e
        with nc.Block() as block:
            @block.tensor
            def _(e):
                for f in self.streams["pe"]:
                    f(e)

            @block.scalar
            def _(e):
                for f in self.streams["act"]:
                    f(e)

            @block.vector
            def _(e):
                for f in self.streams["dve"]:
                    f(e)

            @block.gpsimd
            def _(e):
                for f in self.streams["pool"]:
                    f(e)

            @block.sync
            def _(e):
                for f in self.streams["sp"]:
                    f(e)
        self.es.close()


C_ID = 0
C_ONES = 128
C_BONES = 256
C_RT = 384
C_MASKA = 512
C_SWAM = 1024
NCB = 1280
C_IDF = 1280
C_CHM = 1408
C_INVF = 1920
C_EPSR = 1921
C_EPSL = 1922
C_HALFPI = 1923
NCONST = 1928


def make_consts():
    c = np.zeros((128, NCONST), np.float32)
    c[:, C_ID:C_ID + 128] = np.eye(128, dtype=np.float32)
    c[:, C_IDF:C_IDF + 128] = np.eye(128, dtype=np.float32)
    c[:, C_ONES:C_ONES + 128] = 1.0
    c[0:64, C_BONES:C_BONES + 64] = 1.0
    c[64:128, C_BONES + 64:C_BONES + 128] = 1.0
    rt = np.zeros((128, 128), np.float32)
    for blk in (0, 64):
        for d in range(32):
            rt[blk + d + 32, blk + d] = -1.0
            rt[blk + d, blk + d + 32] = 1.0
    c[:, C_RT:C_RT + 128] = rt
    j = np.arange(128)[:, None]
    i = np.arange(128)[None, :]
    ma = ((j // 64 == i // 64) & (j <= i)).astype(np.float32)
    c[:, C_MASKA:C_MASKA + 512] = np.tile(ma, (1, 4))
    c[:, C_SWAM:C_SWAM + 128] = (j > i).astype(np.float32)
    c[:, C_SWAM + 128:C_SWAM + 256] = (j <= i).astype(np.float32)
    chm = np.ones((128, 512), np.float32)
    chm[:, ::64] = 0.0
    c[:, C_CHM:C_CHM + 512] = chm
    invf = (10000.0 ** (-np.arange(32, dtype=np.float32) / np.float32(32))).astype(np.float32)
    c[:, C_INVF] = invf[np.arange(128) % 32]
    c[:, C_EPSR] = RMS_EPS
    c[:, C_EPSL] = LN_EPS
    c[:, C_HALFPI] = math.pi / 2
    return c


class VecPack:
    def __init__(self):
        self.cols = []
        self.idx = {}

    def add(self, name, v128xn):
        self.idx[name] = sum(a.shape[1] for a in self.cols)
        self.cols.append(np.ascontiguousarray(v128xn, dtype=np.float32))

    def add_feat(self, name, vec):
        v = np.asarray(vec, np.float32).reshape(-1, 128).T
        self.add(name, v)

    def build(self):
        return np.ascontiguousarray(np.concatenate(self.cols, axis=1))


def pack_vecs(inp):
    vp = VecPack()
    for l in range(DEPTH):
        vp.add_feat("nmix%d" % l, inp["norm_mix_g"][l])
        vp.add_feat("nmlp%d" % l, inp["norm_mlp_g"][l])
        vp.add_feat("nmem%d" % l, inp["mem_norm_g"][l])
        vp.add_feat("xaq%d" % l, inp["xa_q_norm_g"][l])
        vp.add_feat("xak%d" % l, inp["xa_k_norm_g"][l])
        vp.add_feat("lbl%d" % l, inp["hgrn_lb_logits"][l])
    for i in range(2):
        vp.add_feat("aon%d" % i, inp["a_o_norm_g"][i])
    vp.add_feat("bq", np.tile(inp["b_q_norm_g"][0], 2))
    vp.add_feat("bk", np.tile(inp["b_k_norm_g"][0], 2))
    vp.add_feat("sink", np.repeat(inp["b_sinks"][0], 64))
    vp.add_feat("cb", inp["c_conv_b"][0])
    vp.add_feat("lng", inp["c_ln_g"][0])
    vp.add_feat("lnb", inp["c_ln_b"][0])
    cw = np.asarray(inp["c_conv_w"][0], np.float32)
    vp.add("cw", cw.reshape(31, 8, 128).transpose(2, 1, 0).reshape(128, 8 * 31))
    return vp


def layer_weights(inp, l):
    kind = KINDS[l]
    idx = KINDS[:l].count(kind)
    if kind == 0:
        w = np.asarray(inp["a_w_in"][idx])
        cols = []
        for h in range(8):
            for s in range(4):
                cols.append(np.arange(s * 1024 + h * 128, s * 1024 + (h + 1) * 128))
        cols.append(np.arange(4096, 4608))
        win = w[:, np.concatenate(cols)]
        wout = inp["a_w_out"][idx]
    elif kind == 1:
        win = np.asarray(inp["b_w_in"][idx])
        wout = inp["b_w_out"][idx]
    else:
        w = np.asarray(inp["c_w_in"][idx])
        cols = []
        for blk in range(4):
            cols.append(np.arange(blk * 256, (blk + 1) * 256))
            cols.append(np.arange(1024 + blk * 256, 1024 + (blk + 1) * 256))
        cols.append(np.arange(2048, 2560))
        win = w[:, np.concatenate(cols)]
        wout = inp["c_w_out"][idx]
    return {
        "win%d" % l: np.ascontiguousarray(win, dtype=np.float32),
        "wout%d" % l: np.ascontiguousarray(wout, dtype=np.float32),
        "wup%d" % l: np.ascontiguousarray(inp["mlp_w_up"][l], dtype=np.float32),
        "wdn%d" % l: np.ascontiguousarray(inp["mlp_w_down"][l], dtype=np.float32),
        "wkv%d" % l: np.ascontiguousarray(inp["xa_w_kv"][l], dtype=np.float32),
    }


WIN_COLS = {0: 4608, 1: 1792, 2: 2560}


def build_program(layers, vidx, nv):
    nc = bass.Bass("TRN2", target_bir_lowering=False)
    x_d = nc.dram_tensor("x", [T, D], F32, kind="ExternalInput").ap()
    mem_d = nc.dram_tensor("mem", [256, D], F32, kind="ExternalInput").ap()
    pos_d = nc.dram_tensor("pos", [1, T], I32, kind="ExternalInput").ap()
    cst_d = nc.dram_tensor("consts", [128, NCONST], F32, kind="ExternalInput").ap()
    vec_d = nc.dram_tensor("vecs", [128, nv], F32, kind="ExternalInput").ap()
    y_d = nc.dram_tensor("y", [T, D], F32, kind="ExternalOutput").ap()
    wd = {}
    for l in layers:
        k = KINDS[l]
        wd["win%d" % l] = nc.dram_tensor("win%d" % l, [D, WIN_COLS[k]], F32, kind="ExternalInput").ap()
        wd["wout%d" % l] = nc.dram_tensor("wout%d" % l, [1536, D], F32, kind="ExternalInput").ap()
        wd["wup%d" % l] = nc.dram_tensor("wup%d" % l, [D, 4096], F32, kind="ExternalInput").ap()
        wd["wdn%d" % l] = nc.dram_tensor("wdn%d" % l, [4096, D], F32, kind="ExternalInput").ap()
        wd["wkv%d" % l] = nc.dram_tensor("wkv%d" % l, [D, D], F32, kind="ExternalInput").ap()

    P = Prog(nc)
    xT = P.sbuf([128, 8, T], F32)
    xTt = trks(8, NG)
    hT = P.sbuf([128, 8, T], BF16)
    hTt = trks(NG)
    mixT = P.sbuf([128, 12, T], BF16)
    mixt = trks(12, 16)
    cst_ = P.sbuf([128, NCONST - NCB], F32)
    cstt = Trk()
    cstb = P.sbuf([128, NCB], BF16)
    cstbt = Trk()

    class _CstView:
        def __getitem__(self, key):
            p, sl = key
            return cst_[p, sl.start - NCB:sl.stop - NCB]
    cst = _CstView()
    vec = P.sbuf([128, nv], F32)
    vect = Trk()
    kxa = P.sbuf([128, 4, 256], BF16)
    kxat = Trk()
    vxa = P.sbuf([128, 2, 512], BF16)
    vxat = Trk()
    small = P.sbuf([128, 64], F32)
    smallt = Trk()
    NSLOT = 2
    STG = 1024
    stage = [P.sbuf([128, STG], F32) for _ in range(2)]
    staget = trks(2)
    wslots = [P.sbuf([128, 4096], BF16) for _ in range(NSLOT)]
    wslott = trks(NSLOT)
    SCRW = 6656
    scr = P.sbuf([128, SCRW], F32)
    pb = [P.psum([128, 512], F32) for _ in range(8)]
    pbt = trks(8)
    state = {"bank": 0, "scr_trks": []}

    def V(name, j=0):
        c = vidx[name] + j
        return vec[:, c:c + 1]

    def CC(col):
        return cst[:, col:col + 1]

    POOLS = {"A": [0, 1, 2, 3], "B": [4, 5, 6, 7], "A1": [0], "A1g": [1], "A2": [2, 3, 4], "B3": [5, 6, 7]}

    def nb(pool=None):
        if pool is None:
            b = state["bank"]
            state["bank"] = (b + 1) % 8
            return b
        lst = POOLS[pool]
        k_ = "bank" + pool
        i_ = state.get(k_, 0)
        state[k_] = (i_ + 1) % len(lst)
        return lst[i_]

    class Scr:
        def __init__(self):
            self.off = 0
            self.t = []
            inh = {}
            for t in state["scr_trks"]:
                for d in ([t.w] if t.w else []) + t.r:
                    if inh.get(d[0], 0) < d[1]:
                        inh[d[0]] = d[1]
            self.inh = list(inh.items())

        def get(self, free_shape, dtype):
            n = int(np.prod(free_shape))
            words = n if dtype in (F32, I32) else (n + 1) // 2
            a = scr[:, self.off:self.off + words]
            self.off += words
            assert self.off <= SCRW, ("scratch overflow", self.off)
            if dtype != F32:
                a = a.bitcast(dtype)
            if len(free_shape) == 2:
                a = a.rearrange("p (a b) -> p a b", a=free_shape[0])
            elif len(free_shape) == 3:
                a = a.rearrange("p (a b c) -> p a b c", a=free_shape[0], b=free_shape[1])
            t = Trk()
            t.r = list(self.inh)
            self.t.append(t)
            return a, t

        def release(self, mark=0):
            inh = dict(self.inh)
            for t in self.t:
                for d in ([t.w] if t.w else []) + t.r:
                    if inh.get(d[0], 0) < d[1]:
                        inh[d[0]] = d[1]
            self.inh = list(inh.items())
            self.off = mark

        def close(self):
            state["scr_trks"] = self.t

    def MM(out, lhsT, rhs, start, stop, reads, writes, inc=None):
        if inc is None:
            inc = stop
        P.op("pe", lambda e: e.matmul(out, lhsT, rhs, start=start, stop=stop), reads, writes, inc,
             cost=0.06 + 0.00045 * int(np.prod(rhs.shape[1:])))

    def TRN(out, in_, ident, reads, writes, inc=True):
        P.op("pe", lambda e: e.transpose(out, in_, ident), reads, writes, inc)

    def ACT(out, in_, func, reads, writes, bias=None, scale=None, accum=None):
        kw = {}
        if bias is not None:
            kw["bias"] = bias
        if scale is not None:
            kw["scale"] = scale
        if accum is not None:
            kw["accum_out"] = accum
        P.op("act", lambda e: e.activation(out, in_, func, **kw), reads, writes,
             cost=0.25 + 0.0008 * int(np.prod(out.shape[1:])))

    def TT(eng, out, a, b, op, reads, writes):
        P.op(eng, lambda e: e.tensor_tensor(out, a, b, op), reads, writes,
             cost=(0.1 + 0.0012 * int(np.prod(out.shape[1:]))) * (1.6 if eng == "pool" else 1.0))

    def TS(eng, out, a, s1, s2, op0, op1, reads, writes):
        if s2 is None:
            P.op(eng, lambda e: e.tensor_scalar(out, a, s1, None, op0), reads, writes)
        else:
            P.op(eng, lambda e: e.tensor_scalar(out, a, s1, s2, op0, op1), reads, writes)

    def STT(out, a, s, b, op0, op1, reads, writes):
        P.op("dve", lambda e: e.scalar_tensor_tensor(out, a, s, b, op0, op1), reads, writes,
             cost=0.1 + 0.0012 * int(np.prod(out.shape[1:])))

    def CP(eng, out, in_, reads, writes):
        if eng == "act":
            ACT(out, in_, AF.Copy, reads, writes)
        else:
            P.op(eng, lambda e: e.tensor_copy(out, in_), reads, writes)

    def RECIP(out, in_, reads, writes):
        P.op("dve", lambda e: e.reciprocal(out, in_), reads, writes)

    idb = cstb[:, C_ID:C_ID + 128]
    onesb = cstb[:, C_ONES:C_ONES + 128]
    bonesb = cstb[:, C_BONES:C_BONES + 128]
    rtb = cstb[:, C_RT:C_RT + 128]
    idf = cst[:, C_IDF:C_IDF + 128]

    def gs(g):
        return slice(g * GS, (g + 1) * GS)

    def rstd_from(bank, out, outt, inv_n, epscol, n=GS, extra_reads=()):
        ACT(out, pb[bank][:, :n], AF.Ln, [pbt[bank], cstt] + list(extra_reads), [outt], bias=CC(epscol), scale=inv_n)
        ACT(out, out, AF.Exp, [outt], [outt], scale=-0.5)

    COST = {"pe": 0.12, "act": 0.65, "dve": 0.6, "pool": 1.0, "sp": 0.0}

    def pipeline_gen(n, *gens):
        gens = [g_ if isinstance(g_, tuple) else (g_, i_) for i_, g_ in enumerate(gens)]
        maxlag = max(lag for _, lag in gens)
        for k in range(n + maxlag):
            chains = []
            for gfn, lag in gens:
                if 0 <= k - lag < n:
                    chains.append([gfn(k - lag), 0.0])
            teng = {e: 0.0 for e in ENGS}
            while chains:
                ch = min(chains, key=lambda c_: c_[1])
                P.log = []
                try:
                    next(ch[0])
                except StopIteration:
                    chains.remove(ch)
                for eng, cst_ in P.log:
                    start = max(ch[1], teng[eng])
                    teng[eng] = start + (COST[eng] if cst_ is None else cst_)
                    ch[1] = teng[eng] + 0.15
            P.log = None

    def pipeline(n, stage1, stage2):
        for k in range(n + 1):
            if k < n:
                stage1(k)
            if k >= 1:
                stage2(k - 1)

    wq = []

    def wspec(l):
        k = KINDS[l]
        s = []
        wkv = wd["wkv%d" % l].rearrange("(k p) n -> p k n", p=128)
        win = wd["win%d" % l].rearrange("(k p) n -> p k n", p=128)
        wout = wd["wout%d" % l].rearrange("(k p) n -> p k n", p=128)
        wup = wd["wup%d" % l].rearrange("(k p) n -> p k n", p=128)
        wdn = wd["wdn%d" % l].rearrange("(k p) n -> p k n", p=128)
        pre = [(wkv[:, :, 0:512], 8, 512), (wkv[:, :, 512:1024], 8, 512)]
        ncols = WIN_COLS[k]
        c0 = 0
        if k == 1:
            widths = [512, 512, 256, 512]
        else:
            widths = [512] * (ncols // 512)
        for w_ in widths:
            s.append((win[:, :, c0:c0 + w_], 8, w_))
            c0 += w_
        for ob in range(4):
            s.append((wout[:, :, ob * 256:(ob + 1) * 256], 12, 256))
        m = []
        for hb in range(8):
            m.append((wup[:, :, hb * 512:(hb + 1) * 512], 8, 512))
            m.append((wdn[:, hb * 4:(hb + 1) * 4, :], 4, 1024))
        return pre, s, m

    specs = [wspec(l) for l in layers]
    for i_, (pre_, s_, m_) in enumerate(specs):
        if i_ == 0:
            wq.extend(pre_)
        wq.extend(s_)
        wq.extend(m_[0:4])
        if i_ + 1 < len(specs):
            wq.extend(specs[i_ + 1][0])
        wq.extend(m_[4:])
    wstate = {"next_load": 0, "next_use": 0, "piece": 0}

    def w_prefetch(upto):
        while wstate["next_load"] < min(upto, len(wq)):
            i = wstate["next_load"]
            view, k, n = wq[i]
            slot = i % NSLOT
            dst = wslots[slot][:, 0:k * n].rearrange("p (k n) -> p k n", k=k)
            kk = max(1, STG // n)
            for k0 in range(0, k, kk):
                k1 = min(k, k0 + kk)
                j = wstate["piece"] % 2
                wstate["piece"] += 1
                stv = stage[j][:, 0:(k1 - k0) * n].rearrange("p (k n) -> p k n", k=k1 - k0)
                P.dma("sp", stv, view[:, k0:k1, :], reads=[], writes=[staget[j]])
                P.op("pool", lambda e, o=dst[:, k0:k1, :], i_=stv: e.tensor_copy(o, i_), [staget[j]], [wslott[slot]])
            wstate["next_load"] += 1

    def w_next(k, n, prefetch=True):
        i = wstate["next_use"]
        assert wq[i][1] == k and wq[i][2] == n, (i, wq[i][1:], k, n)
        w_prefetch(i + NSLOT if prefetch else i + 1)
        wstate["next_use"] += 1
        slot = i % NSLOT
        return wslots[slot][:, 0:k * n].rearrange("p (k n) -> p k n", k=k), wslott[slot]

    P.dma("sp", cst_[:], cst_d[:, NCB:NCONST], writes=[cstt])
    P.dma("sp", vec[:], vec_d, writes=[vect])
    sc = Scr()
    ctmp, ctmpt = sc.get([NCB], F32)
    P.dma("sp", ctmp, cst_d[:, 0:NCB], writes=[ctmpt])
    CP("dve", cstb[:], ctmp, [ctmpt], [cstbt])
    w_prefetch(NSLOT)
    xin, xint = [], []
    for i in range(2):
        a, t = sc.get([D], F32)
        xin.append(a)
        xint.append(t)
    for i in range(16):
        s_ = i % 2
        P.dma("sp", xin[s_], x_d[i * 128:(i + 1) * 128, :], writes=[xint[s_]])
        for half in range(2):
            b = nb()
            for c4 in range(4):
                c = half * 4 + c4
                TRN(pb[b][:, c4 * 128:(c4 + 1) * 128], xin[s_][:, c * 128:(c + 1) * 128], idf,
                    [xint[s_], cstt], [pbt[b]], inc=(c4 == 3))
            g = i // 4
            wr = [xTt[half * 4 + c4][g] for c4 in range(4)]
            CP("dve" if half == 0 else "act", xT[:, half * 4:half * 4 + 4, i * 128:(i + 1) * 128],
               pb[b][:].rearrange("p (c t) -> p c t", c=4), [pbt[b]], wr)
    sc.close()

    def prenorm(gname):
        sc = Scr()
        sqs = [sc.get([8, GS], BF16) for _ in range(2)]
        rs = [sc.get([GS], F32) for _ in range(2)]
        banks = {}

        def s1(g):
            sq, sqt = sqs[g % 2]
            for c in range(8):
                ACT(sq[:, c, :], xT[:, c, gs(g)], AF.Square, [xTt[c][g]], [sqt])
            b = nb()
            banks[g] = b
            for c in range(8):
                MM(pb[b][:], onesb, sq[:, c, :], c == 0, c == 7, [sqt, cstbt], [pbt[b]])

        def s2(g):
            r, rt = rs[g % 2]
            rstd_from(banks[g], r, rt, 1.0 / D, C_EPSR)
            for c in range(8):
                STT(hT[:, c, gs(g)], xT[:, c, gs(g)], V(gname, c), r, ALU.mult, ALU.mult,
                    [xTt[c][g], rt, vect], [hTt[g]])
        pipeline(NG, s1, s2)
        sc.close()

    def proj_fm(wt, wtt, c0, g, pool=None):
        b = nb(pool)
        for kc in range(8):
            MM(pb[b][:], wt[:, kc, c0:c0 + 128], hT[:, kc, gs(g)], kc == 0, kc == 7, [wtt, hTt[g]], [pbt[b]])
        return b

    def proj_gen(wt, wtt, c0, g, pool):
        b = nb(pool)
        for kc in range(8):
            MM(pb[b][:], wt[:, kc, c0:c0 + 128], hT[:, kc, gs(g)], kc == 0, kc == 7, [wtt, hTt[g]], [pbt[b]])
            if kc == 3:
                yield
        return b

    def mt_grp(c, g):
        return mixt[c][4 * g:4 * g + 4]

    def xa_prep_p1(l, sc):
        mm_, mmt = sc.get([2, D], F32)
        junk, junkt = sc.get([D], BF16)
        memn, memnt = sc.get([8, 256], BF16)
        ksq, ksqt = sc.get([4, 256], BF16)
        krs, krst = sc.get([4, 256], F32)
        ssq = small[:, 0:2]
        for mt in range(2):
            P.dma("sp", mm_[:, mt, :], mem_d[mt * 128:(mt + 1) * 128, :], writes=[mmt])
        for mt in range(2):
            ACT(junk, mm_[:, mt, :], AF.Square, [mmt], [junkt, smallt], accum=ssq[:, mt:mt + 1])
        ACT(ssq, ssq, AF.Ln, [smallt, cstt], [smallt], bias=CC(C_EPSR), scale=1.0 / D)
        ACT(ssq, ssq, AF.Exp, [smallt], [smallt], scale=-0.5)
        for mt in range(2):
            TS("dve", mm_[:, mt, :], mm_[:, mt, :], ssq[:, mt:mt + 1], None, ALU.mult, None, [mmt, smallt], [mmt])
        gkq = small[:, 2:3]
        TS("dve", gkq, V("xak%d" % l), V("xaq%d" % l), 128.0 ** -0.5, ALU.mult, ALU.mult, [vect], [smallt])
        return (mm_, mmt, memn, memnt, ksq, ksqt, krs, krst, gkq)

    def xa_prep_p2a(l, bufs):
        mm_, mmt, memn, memnt, ksq, ksqt, krs, krst, gkq = bufs
        for c in range(8):
            b = nb()
            for mt in range(2):
                TRN(pb[b][:, mt * 128:(mt + 1) * 128], mm_[:, mt, c * 128:(c + 1) * 128], idf, [mmt, cstt], [pbt[b]],
                    inc=(mt == 1))
            yield
            TS("dve", memn[:, c, :], pb[b][:, 0:256], V("nmem%d" % l, c), None, ALU.mult, None, [pbt[b], vect], [memnt])
            yield

    def xa_prep_p2(l, bufs):
        mm_, mmt, memn, memnt, ksq, ksqt, krs, krst, gkq = bufs
        wk, wkt = w_next(8, 512)
        kb = []
        for h in range(4):
            if h % 2 == 0:
                b = nb()
                kb.append(b)
            for kc in range(8):
                MM(pb[b][:, (h % 2) * 256:(h % 2 + 1) * 256], wk[:, kc, h * 128:(h + 1) * 128], memn[:, kc, :],
                   kc == 0, kc == 7, [wkt, memnt], [pbt[b]])
        for hp in range(2):
            ACT(ksq[:, 2 * hp:2 * hp + 2, :], pb[kb[hp]][:].rearrange("p (a b) -> p a b", a=2), AF.Square,
                [pbt[kb[hp]]], [ksqt])
        for hp in range(2):
            b = nb()
            MM(pb[b][:], onesb, ksq[:, 2 * hp:2 * hp + 2, :], True, True, [ksqt, cstbt], [pbt[b]])
            kr = krs[:, 2 * hp:2 * hp + 2, :]
            ACT(kr, pb[b][:].rearrange("p (a b) -> p a b", a=2), AF.Ln, [pbt[b], cstt], [krst],
                bias=CC(C_EPSR), scale=1.0 / 128)
            ACT(kr, kr, AF.Exp, [krst], [krst], scale=-0.5)
            STT(kxa[:, 2 * hp:2 * hp + 2, :], pb[kb[hp]][:].rearrange("p (a b) -> p a b", a=2), gkq, kr,
                ALU.mult, ALU.mult, [pbt[kb[hp]], krst, smallt], [kxat])
        wv, wvt = w_next(8, 512)
        for mt in range(2):
            b = nb()
            for kc in range(8):
                MM(pb[b][:], memn[:, kc, mt * 128:(mt + 1) * 128], wv[:, kc, :], kc == 0, kc == 7,
                   [wvt, memnt], [pbt[b]])
            CP("act", vxa[:, mt, :], pb[b][:], [pbt[b]], [vxat])

    def xa_attend(sc, wt, wtt):
        sqs = [sc.get([GS], BF16) for _ in range(2)]
        rss = [sc.get([GS], F32) for _ in range(2)]
        ee = [sc.get([2, 256], BF16) for _ in range(2)]
        rds = [sc.get([256], F32) for _ in range(2)]
        st = {}

        def q1(k):
            h, g = divmod(k, NG)
            sq, sqt = sqs[k % 2]
            bq = proj_fm(wt, wtt, h * 128, g)
            ACT(sq, pb[bq][:], AF.Square, [pbt[bq]], [sqt])
            bs = nb()
            MM(pb[bs][:], onesb, sq, True, True, [sqt, cstbt], [pbt[bs]])
            st[k] = (bq, bs)

        def q2(k):
            h, g = divmod(k, NG)
            bq, bs = st[k]
            rs, rst = rss[k % 2]
            rstd_from(bs, rs, rst, 1.0 / 128, C_EPSR)
            TT("dve", mixT[:, 8 + h, gs(g)], pb[bq][:], rs, ALU.mult, [pbt[bq], rst], mt_grp(8 + h, g))
        pipeline(16, q1, q2)

        def a1(k):
            h, r_ = divmod(k, 8)
            cols = slice(r_ * 256, (r_ + 1) * 256)
            qt = mixt[8 + h][2 * r_:2 * r_ + 2]
            e, et = ee[k % 2]
            b = nb()
            for mt in range(2):
                MM(pb[b][:, mt * 256:(mt + 1) * 256], kxa[:, h, mt * 128:(mt + 1) * 128], mixT[:, 8 + h, cols], True, True,
                   [kxat] + qt, [pbt[b]], inc=(mt == 1))
            ACT(e, pb[b][:].rearrange("p (a b) -> p a b", a=2), AF.Exp, [pbt[b]], [et])

        def a2(k):
            h, r_ = divmod(k, 8)
            cols = slice(r_ * 256, (r_ + 1) * 256)
            qt = mixt[8 + h][2 * r_:2 * r_ + 2]
            e, et = ee[k % 2]
            rd, rdt = rds[k % 2]
            bn = nb()
            for mt in range(2):
                MM(pb[bn][:, 0:256], vxa[:, mt, h * 128:(h + 1) * 128], e[:, mt, :], mt == 0, mt == 1, [vxat, et], [pbt[bn]],
                   inc=False)
            for mt in range(2):
                MM(pb[bn][:, 256:512], onesb, e[:, mt, :], mt == 0, mt == 1, [cstbt, et], [pbt[bn]], inc=(mt == 1))
            ACT(rd, pb[bn][:, 256:512], AF.Ln, [pbt[bn]], [rdt])
            ACT(rd, rd, AF.Exp, [rdt], [rdt], scale=-1.0)
            TT("dve", mixT[:, 8 + h, cols], pb[bn][:, 0:256], rd, ALU.mult, [pbt[bn], rdt], qt)
        pipeline(32, a1, a2)

    def out_proj(l):
        for ob in range(4):
            wt, wtt = w_next(12, 256)
            for oo in range(2):
                o = ob * 2 + oo
                for g in range(NG):
                    b = nb()
                    for kc in range(12):
                        MM(pb[b][:], wt[:, kc, oo * 128:(oo + 1) * 128], mixT[:, kc, gs(g)], kc == 0, kc == 11,
                           [wtt] + mt_grp(kc, g), [pbt[b]])
                    TT("dve", xT[:, o, gs(g)], pb[b][:], xT[:, o, gs(g)], ALU.add, [pbt[b], xTt[o][g]], [xTt[o][g]])

    def mlp(l, next_l=None):
        prenorm("nmlp%d" % l)
        sc = Scr()
        rl = [sc.get([GS], F32) for _ in range(3)]
        xbufs = xa_prep_p1(next_l, sc) if next_l is not None else None
        xgen = xa_prep_p2a(next_l, xbufs) if next_l is not None else iter(())

        def xstep():
            try:
                next(xgen)
            except StopIteration:
                pass
        u = 0
        for hb in range(8):
            if hb == 2 and xbufs is not None:
                for _ in xgen:
                    pass
                xa_prep_p2(next_l, xbufs)
            wu, wut = w_next(8, 512)
            ab = (hb % 2) * 4
            for j in range(4):
                for g in range(NG):
                    b = proj_fm(wu, wut, j * 128, g)
                    r, rt = rl[u % 3]
                    u += 1
                    ACT(r, pb[b][:], AF.Relu, [pbt[b]], [rt])
                    TT("pool", mixT[:, ab + j, gs(g)], r, r, ALU.mult, [rt], mt_grp(ab + j, g))
                    if hb >= 1:
                        xstep()
            wdt, wdtt = w_next(4, 1024)
            for o in range(8):
                for g in range(NG):
                    b = nb()
                    for j in range(4):
                        MM(pb[b][:], wdt[:, j, o * 128:(o + 1) * 128], mixT[:, ab + j, gs(g)], j == 0, j == 3,
                           [wdtt] + mt_grp(ab + j, g), [pbt[b]])
                    TT("dve", xT[:, o, gs(g)], pb[b][:], xT[:, o, gs(g)], ALU.add, [pbt[b], xTt[o][g]], [xTt[o][g]])
        sc.close()

    def lb_prep(l):
        e_ = small[:, 24:56].rearrange("p (l c) -> p l c", l=4)
        for ll in range(4):
            ACT(e_[:, ll, :], vec[:, vidx["lbl%d" % ll]:vidx["lbl%d" % ll] + 8], AF.Exp, [vect], [smallt])
        tot = small[:, 56:64]
        TT("dve", tot, e_[:, 0, :], e_[:, 1, :], ALU.add, [smallt], [smallt])
        TT("dve", tot, tot, e_[:, 2, :], ALU.add, [smallt], [smallt])
        TT("dve", tot, tot, e_[:, 3, :], ALU.add, [smallt], [smallt])
        RECIP(tot, tot, [smallt], [smallt])
        lb = small[:, 8:16]
        P.op("dve", lambda e: e.memset(lb, 0.0), [], [smallt])
        for ll in range(1, l + 1):
            TT("dve", lb, lb, e_[:, ll, :], ALU.add, [smallt], [smallt])
        TT("dve", lb, lb, tot, ALU.mult, [smallt], [smallt])
        oml = small[:, 16:24]
        TS("dve", oml, lb, -1.0, 1.0, ALU.mult, ALU.add, [smallt], [smallt])

    def mixer_hgrn(l):
        idx = KINDS[:l].count(0)
        lb_prep(l)
        sc = Scr()
        Fs = [[sc.get([GS], F32) for _ in range(4)] for _ in range(2)]
        qes = [sc.get([GS], BF16) for _ in range(3)]
        kebs = [sc.get([GS], BF16) for _ in range(3)]
        sgts = [sc.get([GS], BF16) for _ in range(3)]
        xflat = mixT[:, 8:12, :].rearrange("p c t -> p (c t)").bitcast(F32)
        xinh = []
        for c_ in range(8, 12):
            for m_ in mixt[c_]:
                xinh.extend(([m_.w] if m_.w else []) + m_.r)
        xst = {"off": 0, "t": []}

        def xget(free_shape, dtype):
            n_ = int(np.prod(free_shape))
            words = n_ if dtype == F32 else (n_ + 1) // 2
            a_ = xflat[:, xst["off"]:xst["off"] + words]
            xst["off"] += words
            assert xst["off"] <= 4096
            if dtype != F32:
                a_ = a_.bitcast(dtype)
            if len(free_shape) == 2:
                a_ = a_.rearrange("p (a b) -> p a b", a=free_shape[0])
            t_ = Trk()
            t_.r = list(xinh)
            xst["t"].append(t_)
            return a_, t_
        Sall, _ = xget([9, 128], F32)
        Sallt = [Trk() for _ in range(9)]
        for t_ in Sallt:
            t_.r = list(xinh)
            xst["t"].append(t_)
        k2ts = [xget([4, 128], BF16) for _ in range(3)]
        vtks = [xget([4, 128], BF16) for _ in range(3)]
        ees = [xget([16], F32) for _ in range(3)]
        R1, R1t = xget([GS], F32)
        am, amt = xget([GS], BF16)
        sta, stat = xget([8, 128], BF16)
        stats = [stat] + [Trk() for _ in range(7)]
        for t_ in stats[1:]:
            t_.r = list(xinh)
            xst["t"].append(t_)
        chm = cst[:, C_CHM:C_CHM + GS]
        maskA = cstb[:, C_MASKA:C_MASKA + GS]
        LN_MIN = math.log(1e-20)
        wts = {}
        banks = {}

        def sA1(k):
            h, g = divmod(k, NG)
            if g == 0:
                wts[h] = w_next(8, 512, prefetch=(h == 0))
            wt, wtt = wts[h]
            (F1, F1t), (F2, F2t), (F3, F3t), (F4, F4t) = Fs[k % 2]
            lbc = small[:, 8 + h:9 + h]
            bf = yield from proj_gen(wt, wtt, 128, g, "A1")
            yield
            ACT(F1, pb[bf][:], AF.Exp, [pbt[bf]], [F1t], scale=-1.0)
            ACT(F2, F1, AF.Ln, [F1t], [F2t], bias=1.0)
            ACT(F3, F1, AF.Ln, [F1t, smallt], [F3t], bias=1.0, scale=lbc)
            yield
            STT(F3, F2, -1.0, F3, ALU.mult, ALU.add, [F2t, F3t], [F3t])
            TS("dve", F3, F3, LN_MIN, None, ALU.max, None, [F3t], [F3t])
            STT(F1, pb[bf][:], -1.0, F2, ALU.mult, ALU.subtract, [pbt[bf], F2t], [F1t])
            yield
            ACT(F1, F1, AF.Exp, [F1t], [F1t])

        def sA1g(k):
            h, g = divmod(k, NG)
            wt, wtt = wts[h]
            (F1, F1t), (F2, F2t), (F3, F3t), (F4, F4t) = Fs[k % 2]
            sgt_, sgtt = sgts[k % 3]
            bg = yield from proj_gen(wt, wtt, 384, g, "A1g")
            yield
            ACT(F4, pb[bg][:], AF.Exp, [pbt[bg]], [F4t], scale=-1.0)
            yield
            ACT(F4, F4, AF.Ln, [F4t], [F4t], bias=1.0)
            yield
            ACT(F4, F4, AF.Exp, [F4t], [F4t], scale=-1.0)
            yield
            STT(sgt_, pb[bg][:], V("aon%d" % idx, h), F4, ALU.mult, ALU.mult, [pbt[bg], F4t, vect], [sgtt])

        def sA2(k):
            h, g = divmod(k, NG)
            wt, wtt = wts[h]
            (F1, F1t), (F2, F2t), (F3, F3t), (F4, F4t) = Fs[k % 2]
            qe, qet = qes[k % 3]
            keb, kebt = kebs[k % 3]
            k2t, k2tt = k2ts[k % 3]
            vtk, vtkt = vtks[k % 3]
            ee, eet = ees[k % 3]
            omc = small[:, 16 + h:17 + h]
            P.op("dve", lambda e: e.tensor_tensor_scan(F2, chm, F3, 0.0, ALU.mult, ALU.add), [cstt, F3t], [F2t])
            b3 = F2.rearrange("p (c t) -> p c t", c=8)
            bp3 = F3.rearrange("p (c t) -> p c t", c=8)
            TT("dve", bp3, b3, b3[:, :, 31:32].to_broadcast([128, 8, 64]), ALU.subtract, [F2t], [F3t])
            yield
            bq = yield from proj_gen(wt, wtt, 0, g, "A2")
            yield
            ACT(F4, F3, AF.Exp, [F3t], [F4t])
            ACT(ee[:, 0:8], b3[:, :, 63], AF.Exp, [F2t], [eet])
            ACT(ee[:, 8:16], b3[:, :, 31], AF.Exp, [F2t], [eet])
            ACT(F2, F3, AF.Exp, [F3t], [F2t], scale=-1.0)
            yield
            bv = nb("A2")
            for i in range(4):
                for kc in range(8):
                    MM(pb[bv][:, i * 128:(i + 1) * 128], hT[:, kc, g * GS + i * 128:g * GS + (i + 1) * 128],
                       wt[:, kc, 256:384], kc == 0, kc == 7, [wtt, hTt[g]], [pbt[bv]], inc=(kc == 7 and i == 3))
                yield
            TT("dve", qe, pb[bq][:], F4, ALU.mult, [pbt[bq], F4t], [qet])
            STT(keb, F1, omc, F2, ALU.mult, ALU.mult, [F1t, F2t, smallt], [kebt])
            yield
            eb3 = F4.rearrange("p (c t) -> p c t", c=8)
            TT("pool", F3.rearrange("p (c t) -> p c t", c=8), keb.rearrange("p (c t) -> p c t", c=8),
               eb3[:, :, 63:64].to_broadcast([128, 8, 64]), ALU.mult, [kebt, F4t], [F3t])
            CP("act", vtk, pb[bv][:].rearrange("p (a b) -> p a b", a=4), [pbt[bv]], [vtkt])
            yield
            bt = nb("A2")
            for i in range(4):
                TRN(pb[bt][:, i * 128:(i + 1) * 128], F3[:, i * 128:(i + 1) * 128], idf, [F3t, cstt], [pbt[bt]],
                    inc=(i == 3))
            yield
            CP("act", k2t, pb[bt][:].rearrange("p (a b) -> p a b", a=4), [pbt[bt]], [k2tt])
            if g == NG - 1:
                w_prefetch(wstate["next_use"] + 1)

        def sB(k):
            h, g = divmod(k, NG)
            qe, qet = qes[k % 3]
            keb, kebt = kebs[k % 3]
            sgt_, sgtt = sgts[k % 3]
            k2t, k2tt = k2ts[k % 3]
            vtk, vtkt = vtks[k % 3]
            ee, eet = ees[k % 3]
            if g == 0:
                P.op("dve", lambda e: e.memset(Sall[:, 0, :], 0.0), [], [Sallt[0]])
            else:
                CP("dve", Sall[:, 0, :], Sall[:, 8, :], [Sallt[8]], [Sallt[0]])
            bk = [nb("B3"), nb("B3")]
            for par in range(2):
                for i in range(4):
                    MM(pb[bk[par]][:, i * 128:(i + 1) * 128], k2t[par * 64:(par + 1) * 64, i, :],
                       vtk[par * 64:(par + 1) * 64, i, :], True, True, [k2tt, vtkt], [pbt[bk[par]]], inc=(i == 3))
            yield
            ba = nb("B3")
            for i in range(4):
                MM(pb[ba][:, i * 128:(i + 1) * 128], keb[:, i * 128:(i + 1) * 128], qe[:, i * 128:(i + 1) * 128],
                   True, True, [kebt, qet], [pbt[ba]], inc=(i == 3))
            yield
            for cc in range(8):
                ACT(sta[:, cc, :], Sall[:, cc, :], AF.Copy, [Sallt[cc], eet], [stats[cc]], scale=ee[:, 8 + cc:9 + cc])
                STT(Sall[:, cc + 1, :], Sall[:, cc, :], ee[:, cc:cc + 1],
                    pb[bk[cc % 2]][:, (cc // 2) * 128:(cc // 2 + 1) * 128],
                    ALU.mult, ALU.add, [Sallt[cc], eet, pbt[bk[cc % 2]]], [Sallt[cc + 1]])
                if cc == 1:
                    TT("dve", am, pb[ba][:], maskA, ALU.mult, [pbt[ba], cstbt], [amt])
                yield
            bo = nb("B3")
            for i in range(4):
                MM(pb[bo][:, i * 128:(i + 1) * 128], vtk[:, i, :], am[:, i * 128:(i + 1) * 128], True, False,
                   [vtkt, amt], [pbt[bo]], inc=False)
                for hh in range(2):
                    cc = 2 * i + hh
                    MM(pb[bo][:, cc * 64:(cc + 1) * 64], sta[:, cc, :], qe[:, cc * 64:(cc + 1) * 64], False, hh == 1,
                       [stats[cc], qet], [pbt[bo]], inc=(hh == 1 and i == 3))
            yield
            ACT(am, pb[bo][:], AF.Square, [pbt[bo]], [amt])
            yield
            bs = nb("B3")
            MM(pb[bs][:], onesb, am, True, True, [amt, cstbt], [pbt[bs]])
            yield
            rstd_from(bs, R1, R1t, 1.0 / 128, C_EPSR)
            yield
            TT("dve", R1, pb[bo][:], R1, ALU.mult, [pbt[bo], R1t], [R1t])
            TT("dve", mixT[:, h, gs(g)], R1, sgt_, ALU.mult, [R1t, sgtt], mt_grp(h, g))

        pipeline_gen(32, (sA1, 0), (sA1g, 0), (sA2, 1), (sB, 2))
        for t_ in xst["t"]:
            for c_ in range(8, 12):
                for m_ in mixt[c_]:
                    m_.r.extend(([t_.w] if t_.w else []) + t_.r)
        wt, wtt = w_next(8, 512)
        sc.release(0)
        xa_attend(sc, wt, wtt)
        sc.close()

    def mixer_swa(l):
        sc = Scr()
        kT = [(mixT[:, 8, :], Trk()), (mixT[:, 9, :], Trk())]
        vtk, vtkt = mixT[:, 10, :].rearrange("p (a b) -> p a b", a=16), Trk()
        cosT, cost = mixT[:, 11, :], Trk()
        for t_ in (kT[0][1], kT[1][1], vtkt, cost):
            for c_ in range(8, 12):
                for m_ in mixt[c_]:
                    t_.r.extend(([m_.w] if m_.w else []) + m_.r)
        sinT, sint = sc.get([T], BF16)
        esk = small[:, 8:16]
        ACT(esk, vec[:, vidx["sink"]:vidx["sink"] + 8], AF.Exp, [vect], [smallt])
        sc_mark = sc.off
        pi_, pit = sc.get([GS], I32)
        a1, a1t = sc.get([GS], F32)
        a2, a2t = sc.get([GS], F32)
        a3, a3t = sc.get([GS], F32)
        MAGIC = 12582912.0
        C1 = 6.28125
        C2 = TWO_PI - 6.28125
        for g in range(NG):
            P.dma("sp", pi_, pos_d[0:1, gs(g)].partition_broadcast(128), writes=[pit])
            CP("dve", a1, pi_, [pit], [a1t])
            TS("dve", a1, a1, CC(C_INVF), None, ALU.mult, None, [a1t, cstt], [a1t])
            for which, dst, dstt in ((0, sinT, sint), (1, cosT, cost)):
                if which == 1:
                    TS("dve", a1, a1, CC(C_HALFPI), None, ALU.add, None, [a1t, cstt], [a1t])
                TS("dve", a2, a1, 1.0 / TWO_PI, MAGIC, ALU.mult, ALU.add, [a1t], [a2t])
                TS("dve", a2, a2, -MAGIC, None, ALU.add, None, [a2t], [a2t])
                STT(a3, a2, -C1, a1, ALU.mult, ALU.add, [a2t, a1t], [a3t])
                STT(a3, a2, -C2, a3, ALU.mult, ALU.add, [a2t, a3t], [a3t])
                TS("dve", a3, a3, math.pi, -math.pi, ALU.min, ALU.max, [a3t], [a3t])
                ACT(dst[:, gs(g)], a3, AF.Sin, [a3t], [dstt])
        sc.release(sc_mark)
        sqs = [sc.get([GS], BF16) for _ in range(2)]
        qgs = [sc.get([GS], BF16) for _ in range(2)]
        rss = [sc.get([GS], F32) for _ in range(2)]
        t1s = [sc.get([GS], F32) for _ in range(2)]
        t2s = [sc.get([GS], F32) for _ in range(2)]
        units = []
        for tix in range(2):
            for cj in range(4):
                for g in range(NG):
                    units.append(("q", tix, cj, g))
        for gk in range(2):
            for g in range(NG):
                units.append(("k", gk, 0, g))
        wcache = {}
        st = {}

        def get_w(key, k_, n_):
            if key not in wcache:
                wcache[key] = w_next(k_, n_)
            return wcache[key]

        def r1(k):
            kind_, a_, cj, g = units[k]
            if kind_ == "q":
                wt, wtt = get_w(("q", a_), 8, 512)
                b = proj_fm(wt, wtt, cj * 128, g)
                gcol = V("bq")
            else:
                wt, wtt = get_w("kv", 8, 256)
                b = nb()
                for rep in range(2):
                    for kc in range(8):
                        MM(pb[b][rep * 64:(rep + 1) * 64, :], wt[:, kc, a_ * 64:(a_ + 1) * 64], hT[:, kc, gs(g)],
                           kc == 0, kc == 7, [wtt, hTt[g]], [pbt[b]], inc=(kc == 7 and rep == 1))
                gcol = V("bk")
            sq, sqt = sqs[k % 2]
            qg, qgt = qgs[k % 2]
            ACT(sq, pb[b][:], AF.Square, [pbt[b]], [sqt])
            ACT(qg, pb[b][:], AF.Copy, [pbt[b], vect], [qgt], scale=gcol)
            bm = nb()
            MM(pb[bm][:], bonesb, sq, True, True, [sqt, cstbt], [pbt[bm]])
            br = nb()
            MM(pb[br][:], rtb, qg, True, True, [qgt, cstbt], [pbt[br]])
            st[k] = (bm, br)

        def r2(k):
            kind_, a_, cj, g = units[k]
            bm, br = st[k]
            qg, qgt = qgs[k % 2]
            rs, rst = rss[k % 2]
            t1, t1t = t1s[k % 2]
            t2, t2t = t2s[k % 2]
            if kind_ == "q":
                c = a_ * 4 + cj
                dst, dstt_list = mixT[:, c, gs(g)], mt_grp(c, g)
            else:
                dst, dstt_list = kT[a_][0][:, gs(g)], [kT[a_][1]]
            rstd_from(bm, rs, rst, 1.0 / 64, C_EPSR)
            TT("pool", t1, qg, cosT[:, gs(g)], ALU.mult, [qgt, cost], [t1t])
            TT("dve", t2, pb[br][:], sinT[:, gs(g)], ALU.mult, [pbt[br], sint], [t2t])
            TT("dve", t1, t1, t2, ALU.add, [t1t, t2t], [t1t])
            TT("dve", dst, t1, rs, ALU.mult, [t1t, rst], dstt_list)
        pipeline(len(units), r1, r2)
        wt, wtt = get_w("kv", 8, 256)
        for i4 in range(4):
            b = nb()
            for ii in range(4):
                i = i4 * 4 + ii
                for kc in range(8):
                    MM(pb[b][:, ii * 128:(ii + 1) * 128], hT[:, kc, i * 128:(i + 1) * 128], wt[:, kc, 128:256],
                       kc == 0, kc == 7, [wtt, hTt[i4]], [pbt[b]], inc=(kc == 7 and ii == 3))
            CP("act", vtk[:, i4 * 4:i4 * 4 + 4, :], pb[b][:].rearrange("p (a b) -> p a b", a=4), [pbt[b]], [vtkt])
        sc.release(sc_mark)
        EE = []
        for _ in range(2):
            e_, t0_ = sc.get([2, 2, 128], BF16)
            t1_ = Trk()
            t1_.r = list(t0_.r)
            sc.t.append(t1_)
            EE.append((e_, (t0_, t1_)))
        dn_ = [sc.get([128], F32) for _ in range(2)]
        swam = cstb[:, C_SWAM:C_SWAM + 256].rearrange("p (a b) -> p a b", a=2)

        def c1(u):
            c, n = divmod(u, 16)
            gk = c // 4
            kTa, kTt = kT[gk]
            kbs = [1] if n == 0 else [0, 1]
            e, et = EE[u % 2]
            q_t = [mixt[c][n]]
            for par in range(2):
                b = nb("A")
                for kb_ in kbs:
                    nbk = n - 1 + kb_
                    MM(pb[b][:, kb_ * 128:(kb_ + 1) * 128], kTa[par * 64:(par + 1) * 64, nbk * 128:(nbk + 1) * 128],
                       mixT[par * 64:(par + 1) * 64, c, n * 128:(n + 1) * 128], True, True, [kTt] + q_t, [pbt[b]],
                       inc=(kb_ == 1))
                lo = kbs[0]
                ACT(e[:, par, lo:2, :], pb[b][:, lo * 128:256].rearrange("p (a b) -> p a b", b=128), AF.Exp,
                    [pbt[b]], [et[par]], scale=0.125)
                TT("pool" if par == 0 else "dve", e[:, par, lo:2, :], e[:, par, lo:2, :], swam[:, lo:2, :], ALU.mult,
                   [et[par], cstbt], [et[par]])
                yield

        def c2(u):
            c, n = divmod(u, 16)
            gk = c // 4
            kbs = [1] if n == 0 else [0, 1]
            e, et = EE[u % 2]
            dn, dnt = dn_[u % 2]
            bn = nb("B")
            for par in range(2):
                for which in range(2):
                    for kb_ in kbs:
                        nbk = n - 1 + kb_
                        lhs = vtk[:, nbk, gk * 64:(gk + 1) * 64] if which == 0 else onesb[:, 0:64]
                        MM(pb[bn][par * 64:(par + 1) * 64, which * 128:(which + 1) * 128], lhs, e[:, par, kb_, :],
                           kb_ == kbs[0], kb_ == 1, [vtkt, cstbt, et[par]], [pbt[bn]],
                           inc=(kb_ == 1 and which == 1 and par == 1))
            yield
            ACT(dn, pb[bn][:, 128:256], AF.Ln, [pbt[bn], smallt], [dnt], bias=esk[:, c:c + 1])
            ACT(dn, dn, AF.Exp, [dnt], [dnt], scale=-1.0)
            yield
            TT("dve", mixT[:, c, n * 128:(n + 1) * 128], pb[bn][:, 0:128], dn, ALU.mult, [pbt[bn], dnt], [mixt[c][n]])
        pipeline_gen(128, c1, c2)
        for t_ in (kT[0][1], kT[1][1], vtkt, cost):
            for c_ in range(8, 12):
                for m_ in mixt[c_]:
                    m_.r.extend(([t_.w] if t_.w else []) + t_.r)
        wt, wtt = w_next(8, 512)
        sc.release(0)
        xa_attend(sc, wt, wtt)
        sc.close()

    def mixer_conv(l):
        sc = Scr()
        UW = 30 + T
        uT = [sc.get([UW], BF16) for _ in range(2)]
        dgs = [sc.get([31, 128], BF16) for _ in range(2)]
        sg = [sc.get([GS], BF16) for _ in range(2)]
        cwv = vec[:, vidx["cw"]:vidx["cw"] + 8 * 31].rearrange("p (c w) -> p c w", c=8)
        for u_, ut_ in uT:
            P.op("pool", lambda e, u_=u_: e.memset(u_[:, 0:30], 0.0), [], [ut_])
        su = 0
        wts = {}

        def build_dg(c):
            dg, dgt = dgs[c % 2]
            for w_ in range(31):
                ACT(dg[:, w_, :], idb, AF.Copy, [cstbt, vect], [dgt], scale=cwv[:, c, w_:w_ + 1])

        def glu(c):
            blk, j = divmod(c, 2)
            if blk not in wts:
                wts[blk] = w_next(8, 512)
            wt, wtt = wts[blk]
            u_, ut_ = uT[c % 2]
            for g in range(NG):
                ba = proj_fm(wt, wtt, j * 128, g)
                bg = proj_fm(wt, wtt, 256 + j * 128, g)
                s_, st_ = sg[(c * NG + g) % 2]
                ACT(s_, pb[bg][:], AF.Sigmoid, [pbt[bg]], [st_])
                TT("dve", u_[:, 30 + g * GS:30 + (g + 1) * GS], pb[ba][:], s_, ALU.mult, [pbt[ba], st_], [ut_])

        def conv(c):
            u_, ut_ = uT[c % 2]
            dg, dgt = dgs[c % 2]
            for g in range(NG):
                b = nb()
                for w_ in range(31):
                    MM(pb[b][:], dg[:, w_, :], u_[:, g * GS + w_:g * GS + w_ + GS], w_ == 0, w_ == 30,
                       [dgt, ut_], [pbt[b]])
                ACT(mixT[:, c, gs(g)], pb[b][:], AF.Identity, [pbt[b], vect], mt_grp(c, g), bias=V("cb", c))

        for c in range(9):
            if c < 8:
                build_dg(c)
                glu(c)
            if c >= 1:
                conv(c - 1)
        sc.release(0)
        ysqs = [sc.get([GS], BF16) for _ in range(2)]
        mus = [sc.get([GS], F32) for _ in range(2)]
        m2s = [sc.get([GS], F32) for _ in range(2)]
        rss = [sc.get([GS], F32) for _ in range(2)]
        tt_ = [sc.get([GS], F32) for _ in range(2)]
        st = {}

        def l1(g):
            ysq, ysqt = ysqs[g % 2]
            b1 = nb()
            for c in range(8):
                MM(pb[b1][:], onesb, mixT[:, c, gs(g)], c == 0, c == 7, [cstbt] + mt_grp(c, g), [pbt[b1]])
            b2 = nb()
            for c in range(8):
                ACT(ysq, mixT[:, c, gs(g)], AF.Square, mt_grp(c, g), [ysqt])
                MM(pb[b2][:], onesb, ysq, c == 0, c == 7, [cstbt, ysqt], [pbt[b2]], inc=True)
            st[g] = (b1, b2)

        def l2(g):
            b1, b2 = st[g]
            mu, mut = mus[g % 2]
            m2, m2t = m2s[g % 2]
            rs, rst = rss[g % 2]
            ACT(mu, pb[b1][:], AF.Copy, [pbt[b1]], [mut], scale=1.0 / D)
            TT("pool", m2, mu, mu, ALU.mult, [mut], [m2t])
            STT(m2, pb[b2][:], 1.0 / D, m2, ALU.mult, ALU.subtract, [pbt[b2], m2t], [m2t])
            ACT(rs, m2, AF.Ln, [m2t, cstt], [rst], bias=CC(C_EPSL), scale=1.0)
            ACT(rs, rs, AF.Exp, [rst], [rst], scale=-0.5)
            for c in range(8):
                t_, ttt = tt_[c % 2]
                TT("dve", t_, mixT[:, c, gs(g)], mu, ALU.subtract, mt_grp(c, g) + [mut], [ttt])
                TT("dve", t_, t_, rs, ALU.mult, [ttt, rst], [ttt])
                ACT(mixT[:, c, gs(g)], t_, AF.Silu, [ttt, vect], mt_grp(c, g), bias=V("lnb", c), scale=V("lng", c))
        pipeline(NG, l1, l2)
        wt, wtt = w_next(8, 512)
        sc.release(0)
        xa_attend(sc, wt, wtt)
        sc.close()

    for li, l in enumerate(layers):
        if li == 0:
            sc0 = Scr()
            xb0 = xa_prep_p1(l, sc0)
            for _ in xa_prep_p2a(l, xb0):
                pass
            xa_prep_p2(l, xb0)
            sc0.close()
        prenorm("nmix%d" % l)
        kind = KINDS[l]
        if kind == 0:
            mixer_hgrn(l)
        elif kind == 1:
            mixer_swa(l)
        else:
            mixer_conv(l)
        out_proj(l)
        mlp(l, layers[li + 1] if li + 1 < len(layers) else None)

    sc = Scr()
    yo = [sc.get([D], F32) for _ in range(2)]
    out_toks = []
    for i in range(16):
        y_, yt_ = yo[i % 2]
        g = i // 4
        for half in range(2):
            b = nb()
            for c4 in range(4):
                c = half * 4 + c4
                TRN(pb[b][:, c4 * 128:(c4 + 1) * 128], xT[:, c, i * 128:(i + 1) * 128], idf, [xTt[c][g], cstt], [pbt[b]],
                    inc=(c4 == 3))
            CP("dve" if half == 0 else "act", y_[:, half * 512:(half + 1) * 512], pb[b][:], [pbt[b]], [yt_])
        out_toks.append(P.dma("sp", y_d[i * 128:(i + 1) * 128, :], y_, reads=[yt_]))
    sc.close()
    P.wait_tok("sp", out_toks)
    P.emit()
    return nc


_CACHE = {}


def run_layers(inp, x, layers):
    vp = pack_vecs(inp)
    vecs = vp.build()
    key = tuple(layers)
    if key not in _CACHE:
        _CACHE[key] = build_program(list(layers), vp.idx, vecs.shape[1])
    nc = _CACHE[key]
    consts = make_consts()
    wts = {}
    for l in layers:
        wts.update(layer_weights(inp, l))
    mem = np.asarray(inp["mem"], np.float32)
    pos = np.asarray(inp["positions"], np.int32)
    in_maps = []
    for b in range(8):
        m = {"x": np.ascontiguousarray(x[b]), "mem": np.ascontiguousarray(mem[b]),
             "pos": np.ascontiguousarray(pos[b:b + 1]), "consts": consts, "vecs": vecs}
        m.update(wts)
        in_maps.append(m)
    res = run_bass_kernel_spmd(nc, in_maps, core_ids=list(range(8)))
    return np.stack([np.asarray(r["y"], np.float32) for r in res.results], axis=0)


LAUNCH_PLAN = [[0, 1, 2, 3]]


def kernel(**inputs):
    x = np.asarray(inputs["x"], np.float32)
    for group in LAUNCH_PLAN:
        x = run_layers(inputs, x, group)
    return x
```

```python
import contextlib
import math
import numpy as np
import concourse.bass as bass
import concourse.mybir as mybir
from concourse.bass_utils import run_bass_kernel_spmd

F32 = mybir.dt.float32
BF16 = mybir.dt.bfloat16
I32 = mybir.dt.int32
AF = mybir.ActivationFunctionType
ALU = mybir.AluOpType

ENGS = ("pe", "act", "dve", "pool", "sp")
D = 1024
T = 2048
NG = 4
GS = 512
DEPTH = 4
KINDS = (0, 1, 2, 0)
RMS_EPS = 1e-6
LN_EPS = 1e-5
TWO_PI = 2.0 * math.pi


class Trk:
    __slots__ = ("w", "r")

    def __init__(self):
        self.w = None
        self.r = []


def trks(*shape):
    if len(shape) == 1:
        return [Trk() for _ in range(shape[0])]
    return [trks(*shape[1:]) for _ in range(shape[0])]


class Prog:
    def __init__(self, nc, n_dma_sems=32):
        self.nc = nc
        self.es = contextlib.ExitStack()
        self.streams = {e: [] for e in ENGS}
        self.count = {e: 0 for e in ENGS}
        self.pending = {e: False for e in ENGS}
        self.waited = {e: {} for e in ENGS}
        self.sems = {}
        for e in ("pe", "act", "dve", "pool"):
            self.sems[e] = self.es.enter_context(nc.semaphore("s_" + e))
        self.dma_sems = []
        for i in range(n_dma_sems):
            k = "d%d" % i
            self.sems[k] = self.es.enter_context(nc.semaphore("s_" + k))
            self.dma_sems.append(k)
        self.dma_val = {k: 0 for k in self.dma_sems}
        self.dma_rr = 0
        self.ntens = 0
        self.NSW = 4
        self.sw_sems = [self.es.enter_context(nc.semaphore("s_sw%d" % i)) for i in range(self.NSW)]
        self.sems["rel"] = self.es.enter_context(nc.semaphore("s_rel"))
        self.sw_issued = 0
        self.sw_relayed = 0

    def sbuf(self, shape, dtype):
        self.ntens += 1
        return self.es.enter_context(self.nc.sbuf_tensor("t%d" % self.ntens, list(shape), dtype))

    def psum(self, shape, dtype):
        self.ntens += 1
        return self.es.enter_context(self.nc.psum_tensor("p%d" % self.ntens, list(shape), dtype))

    @staticmethod
    def _deps(reads, writes):
        deps = {}

        def add(d):
            if d is not None and deps.get(d[0], 0) < d[1]:
                deps[d[0]] = d[1]
        for t in reads:
            add(t.w)
        for t in writes:
            add(t.w)
            for d in t.r:
                add(d)
        return deps

    def _emit_waits(self, eng, deps):
        for k, v in deps.items():
            if k == eng:
                if eng == "pe" or v <= self.count[eng] - 10 or v > self.count[eng]:
                    continue
            if self.waited[eng].get(k, 0) >= v:
                continue
            self.waited[eng][k] = v
            sem = self.sems[k]
            self.streams[eng].append(lambda e, sem=sem, v=v: e.wait_ge(sem, v))

    def _mark(self, tok, reads, writes):
        for t in reads:
            t.r.append(tok)
            if len(t.r) > 64:
                m = {}
                for k, v in t.r:
                    if m.get(k, 0) < v:
                        m[k] = v
                t.r = list(m.items())
        for t in writes:
            t.w = tok
            t.r = []

    log = None

    def op(self, eng, fn, reads=(), writes=(), inc=True, cost=None):
        if self.log is not None:
            self.log.append((eng, cost))
        self._emit_waits(eng, self._deps(reads, writes))
        tok = (eng, self.count[eng] + 1)
        if inc:
            sem = self.sems[eng]
            self.streams[eng].append(lambda e, fn=fn, sem=sem: fn(e).then_inc(sem, 1))
            self.count[eng] += 1
            self.pending[eng] = False
        else:
            self.streams[eng].append(lambda e, fn=fn: fn(e))
            self.pending[eng] = True
        self._mark(tok, reads, writes)
        return tok

    def dma(self, q, out, in_, reads=(), writes=()):
        deps = self._deps(reads, writes)
        k = self.dma_sems[self.dma_rr % len(self.dma_sems)]
        self.dma_rr += 1
        if self.dma_val[k] > 0:
            deps[k] = max(deps.get(k, 0), self.dma_val[k])
        self._emit_waits(q, deps)
        self.dma_val[k] += 16
        tok = (k, self.dma_val[k])
        sem = self.sems[k]
        self.streams[q].append(lambda e, out=out, in_=in_, sem=sem: e.dma_start(out=out, in_=in_).then_inc(sem, 16))
        self._mark(tok, reads, writes)
        return tok

    def dma_sw(self, out, in_, reads=(), writes=()):
        i = self.sw_issued
        if i - self.sw_relayed >= self.NSW:
            self.relay_upto(i - self.NSW)
        self._emit_waits("pool", self._deps(reads, writes))
        sem = self.sw_sems[i % self.NSW]
        self.streams["pool"].append(lambda e, out=out, in_=in_, sem=sem: e.dma_start(out=out, in_=in_).then_inc(sem, 16))
        self.sw_issued += 1
        self._mark(("rel", i + 1), reads, writes)
        return i

    def relay_upto(self, i):
        rel = self.sems["rel"]
        while self.sw_relayed <= min(i, self.sw_issued - 1):
            sem = self.sw_sems[self.sw_relayed % self.NSW]

            def f(e, sem=sem, rel=rel):
                e.wait_ge(sem, 16)
                e.sem_inc(sem, -16)
                e.sem_inc(rel, 1)
            self.streams["pool"].append(f)
            self.sw_relayed += 1

    def wait_tok(self, eng, toks):
        deps = {}
        for k, v in toks:
            if deps.get(k, 0) < v:
                deps[k] = v
        self._emit_waits(eng, deps)

    def emit(self):
        nc = self.nc
        self.relay_upto(self.sw_issued - 1)
        for e in ENGS:
            assert not self.pending[e], e
        with nc.Block() as block:
            @block.tensor
            def _(e):
                for f in self.streams["pe"]:
                    f(e)

            @block.scalar
            def _(e):
                for f in self.streams["act"]:
                    f(e)

            @block.vector
            def _(e):
                for f in self.streams["dve"]:
                    f(e)

            @block.gpsimd
            def _(e):
                for f in self.streams["pool"]:
                    f(e)

            @block.sync
            def _(e):
                for f in self.streams["sp"]:
                    f(e)
        self.es.close()


C_ID = 0
C_ONES = 128
C_BONES = 256
C_RT = 384
C_MASKA = 512
C_SWAM = 1024
NCB = 1280
C_IDF = 1280
C_CHM = 1408
C_INVF = 1920
C_EPSR = 1921
C_EPSL = 1922
C_HALFPI = 1923
NCONST = 1928


def make_consts():
    c = np.zeros((128, NCONST), np.float32)
    c[:, C_ID:C_ID + 128] = np.eye(128, dtype=np.float32)
    c[:, C_IDF:C_IDF + 128] = np.eye(128, dtype=np.float32)
    c[:, C_ONES:C_ONES + 128] = 1.0
    c[0:64, C_BONES:C_BONES + 64] = 1.0
    c[64:128, C_BONES + 64:C_BONES + 128] = 1.0
    rt = np.zeros((128, 128), np.float32)
    for blk in (0, 64):
        for d in range(32):
            rt[blk + d + 32, blk + d] = -1.0
            rt[blk + d, blk + d + 32] = 1.0
    c[:, C_RT:C_RT + 128] = rt
    j = np.arange(128)[:, None]
    i = np.arange(128)[None, :]
    ma = ((j // 64 == i // 64) & (j <= i)).astype(np.float32)
    c[:, C_MASKA:C_MASKA + 512] = np.tile(ma, (1, 4))
    c[:, C_SWAM:C_SWAM + 128] = (j > i).astype(np.float32)
    c[:, C_SWAM + 128:C_SWAM + 256] = (j <= i).astype(np.float32)
    chm = np.ones((128, 512), np.float32)
    chm[:, ::64] = 0.0
    c[:, C_CHM:C_CHM + 512] = chm
    invf = (10000.0 ** (-np.arange(32, dtype=np.float32) / np.float32(32))).astype(np.float32)
    c[:, C_INVF] = invf[np.arange(128) % 32]
    c[:, C_EPSR] = RMS_EPS
    c[:, C_EPSL] = LN_EPS
    c[:, C_HALFPI] = math.pi / 2
    return c


class VecPack:
    def __init__(self):
        self.cols = []
        self.idx = {}

    def add(self, name, v128xn):
        self.idx[name] = sum(a.shape[1] for a in self.cols)
        self.cols.append(np.ascontiguousarray(v128xn, dtype=np.float32))

    def add_feat(self, name, vec):
        v = np.asarray(vec, np.float32).reshape(-1, 128).T
        self.add(name, v)

    def build(self):
        return np.ascontiguousarray(np.concatenate(self.cols, axis=1))


def pack_vecs(inp):
    vp = VecPack()
    for l in range(DEPTH):
        vp.add_feat("nmix%d" % l, inp["norm_mix_g"][l])
        vp.add_feat("nmlp%d" % l, inp["norm_mlp_g"][l])
        vp.add_feat("nmem%d" % l, inp["mem_norm_g"][l])
        vp.add_feat("xaq%d" % l, inp["xa_q_norm_g"][l])
        vp.add_feat("xak%d" % l, inp["xa_k_norm_g"][l])
        vp.add_feat("lbl%d" % l, inp["hgrn_lb_logits"][l])
    for i in range(2):
        vp.add_feat("aon%d" % i, inp["a_o_norm_g"][i])
    vp.add_feat("bq", np.tile(inp["b_q_norm_g"][0], 2))
    vp.add_feat("bk", np.tile(inp["b_k_norm_g"][0], 2))
    vp.add_feat("sink", np.repeat(inp["b_sinks"][0], 64))
    vp.add_feat("cb", inp["c_conv_b"][0])
    vp.add_feat("lng", inp["c_ln_g"][0])
    vp.add_feat("lnb", inp["c_ln_b"][0])
    cw = np.asarray(inp["c_conv_w"][0], np.float32)
    vp.add("cw", cw.reshape(31, 8, 128).transpose(2, 1, 0).reshape(128, 8 * 31))
    return vp


def layer_weights(inp, l):
    kind = KINDS[l]
    idx = KINDS[:l].count(kind)
    if kind == 0:
        w = np.asarray(inp["a_w_in"][idx])
        cols = []
        for h in range(8):
            for s in range(4):
                cols.append(np.arange(s * 1024 + h * 128, s * 1024 + (h + 1) * 128))
        cols.append(np.arange(4096, 4608))
        win = w[:, np.concatenate(cols)]
        wout = inp["a_w_out"][idx]
    elif kind == 1:
        win = np.asarray(inp["b_w_in"][idx])
        wout = inp["b_w_out"][idx]
    else:
        w = np.asarray(inp["c_w_in"][idx])
        cols = []
        for blk in range(4):
            cols.append(np.arange(blk * 256, (blk + 1) * 256))
            cols.append(np.arange(1024 + blk * 256, 1024 + (blk + 1) * 256))
        cols.append(np.arange(2048, 2560))
        win = w[:, np.concatenate(cols)]
        wout = inp["c_w_out"][idx]
    return {
        "win%d" % l: np.ascontiguousarray(win, dtype=np.float32),
        "wout%d" % l: np.ascontiguousarray(wout, dtype=np.float32),
        "wup%d" % l: np.ascontiguousarray(inp["mlp_w_up"][l], dtype=np.float32),
        "wdn%d" % l: np.ascontiguousarray(inp["mlp_w_down"][l], dtype=np.float32),
        "wkv%d" % l: np.ascontiguousarray(inp["xa_w_kv"][l], dtype=np.float32),
    }


WIN_COLS = {0: 4608, 1: 1792, 2: 2560}


def build_program(layers, vidx, nv):
    nc = bass.Bass("TRN2", target_bir_lowering=False)
    x_d = nc.dram_tensor("x", [T, D], F32, kind="ExternalInput").ap()
    mem_d = nc.dram_tensor("mem", [256, D], F32, kind="ExternalInput").ap()
    pos_d = nc.dram_tensor("pos", [1, T], I32, kind="ExternalInput").ap()
    cst_d = nc.dram_tensor("consts", [128, NCONST], F32, kind="ExternalInput").ap()
    vec_d = nc.dram_tensor("vecs", [128, nv], F32, kind="ExternalInput").ap()
    y_d = nc.dram_tensor("y", [T, D], F32, kind="ExternalOutput").ap()
    wd = {}
    for l in layers:
        k = KINDS[l]
        wd["win%d" % l] = nc.dram_tensor("win%d" % l, [D, WIN_COLS[k]], F32, kind="ExternalInput").ap()
        wd["wout%d" % l] = nc.dram_tensor("wout%d" % l, [1536, D], F32, kind="ExternalInput").ap()
        wd["wup%d" % l] = nc.dram_tensor("wup%d" % l, [D, 4096], F32, kind="ExternalInput").ap()
        wd["wdn%d" % l] = nc.dram_tensor("wdn%d" % l, [4096, D], F32, kind="ExternalInput").ap()
        wd["wkv%d" % l] = nc.dram_tensor("wkv%d" % l, [D, D], F32, kind="ExternalInput").ap()

    P = Prog(nc)
    xT = P.sbuf([128, 8, T], F32)
    xTt = trks(8, NG)
    hT = P.sbuf([128, 8, T], BF16)
    hTt = trks(NG)
    mixT = P.sbuf([128, 12, T], BF16)
    mixt = trks(12, 16)
    cst_ = P.sbuf([128, NCONST - NCB], F32)
    cstt = Trk()
    cstb = P.sbuf([128, NCB], BF16)
    cstbt = Trk()

    class _CstView:
        def __getitem__(self, key):
            p, sl = key
            return cst_[p, sl.start - NCB:sl.stop - NCB]
    cst = _CstView()
    vec = P.sbuf([128, nv], F32)
    vect = Trk()
    kxa = P.sbuf([128, 4, 256], BF16)
    kxat = Trk()
    vxa = P.sbuf([128, 2, 512], BF16)
    vxat = Trk()
    small = P.sbuf([128, 64], F32)
    smallt = Trk()
    NSLOT = 2
    STG = 1024
    stage = [P.sbuf([128, STG], F32) for _ in range(2)]
    staget = trks(2)
    wslots = [P.sbuf([128, 4096], BF16) for _ in range(NSLOT)]
    wslott = trks(NSLOT)
    SCRW = 6656
    scr = P.sbuf([128, SCRW], F32)
    pb = [P.psum([128, 512], F32) for _ in range(8)]
    pbt = trks(8)
    state = {"bank": 0, "scr_trks": []}

    def V(name, j=0):
        c = vidx[name] + j
        return vec[:, c:c + 1]

    def CC(col):
        return cst[:, col:col + 1]

    POOLS = {"A": [0, 1, 2, 3], "B": [4, 5, 6, 7], "A1": [0], "A1g": [1], "A2": [2, 3, 4], "B3": [5, 6, 7]}

    def nb(pool=None):
        if pool is None:
            b = state["bank"]
            state["bank"] = (b + 1) % 8
            return b
        lst = POOLS[pool]
        k_ = "bank" + pool
        i_ = state.get(k_, 0)
        state[k_] = (i_ + 1) % len(lst)
        return lst[i_]

    class Scr:
        def __init__(self):
            self.off = 0
            self.t = []
            inh = {}
            for t in state["scr_trks"]:
                for d in ([t.w] if t.w else []) + t.r:
                    if inh.get(d[0], 0) < d[1]:
                        inh[d[0]] = d[1]
            self.inh = list(inh.items())

        def get(self, free_shape, dtype):
            n = int(np.prod(free_shape))
            words = n if dtype in (F32, I32) else (n + 1) // 2
            a = scr[:, self.off:self.off + words]
            self.off += words
            assert self.off <= SCRW, ("scratch overflow", self.off)
            if dtype != F32:
                a = a.bitcast(dtype)
            if len(free_shape) == 2:
                a = a.rearrange("p (a b) -> p a b", a=free_shape[0])
            elif len(free_shape) == 3:
                a = a.rearrange("p (a b c) -> p a b c", a=free_shape[0], b=free_shape[1])
            t = Trk()
            t.r = list(self.inh)
            self.t.append(t)
            return a, t

        def release(self, mark=0):
            inh = dict(self.inh)
            for t in self.t:
                for d in ([t.w] if t.w else []) + t.r:
                    if inh.get(d[0], 0) < d[1]:
                        inh[d[0]] = d[1]
            self.inh = list(inh.items())
            self.off = mark

        def close(self):
            state["scr_trks"] = self.t

    def MM(out, lhsT, rhs, start, stop, reads, writes, inc=None):
        if inc is None:
            inc = stop
        P.op("pe", lambda e: e.matmul(out, lhsT, rhs, start=start, stop=stop), reads, writes, inc,
             cost=0.06 + 0.00045 * int(np.prod(rhs.shape[1:])))

    def TRN(out, in_, ident, reads, writes, inc=True):
        P.op("pe", lambda e: e.transpose(out, in_, ident), reads, writes, inc)

    def ACT(out, in_, func, reads, writes, bias=None, scale=None, accum=None):
        kw = {}
        if bias is not None:
            kw["bias"] = bias
        if scale is not None:
            kw["scale"] = scale
        if accum is not None:
            kw["accum_out"] = accum
        P.op("act", lambda e: e.activation(out, in_, func, **kw), reads, writes,
             cost=0.25 + 0.0008 * int(np.prod(out.shape[1:])))

    def TT(eng, out, a, b, op, reads, writes):
        P.op(eng, lambda e: e.tensor_tensor(out, a, b, op), reads, writes,
             cost=(0.1 + 0.0012 * int(np.prod(out.shape[1:]))) * (1.6 if eng == "pool" else 1.0))

    def TS(eng, out, a, s1, s2, op0, op1, reads, writes):
        if s2 is None:
            P.op(eng, lambda e: e.tensor_scalar(out, a, s1, None, op0), reads, writes)
        else:
            P.op(eng, lambda e: e.tensor_scalar(out, a, s1, s2, op0, op1), reads, writes)

    def STT(out, a, s, b, op0, op1, reads, writes):
        P.op("dve", lambda e: e.scalar_tensor_tensor(out, a, s, b, op0, op1), reads, writes,
             cost=0.1 + 0.0012 * int(np.prod(out.shape[1:])))

    def CP(eng, out, in_, reads, writes):
        if eng == "act":
            ACT(out, in_, AF.Copy, reads, writes)
        else:
            P.op(eng, lambda e: e.tensor_copy(out, in_), reads, writes)

    def RECIP(out, in_, reads, writes):
        P.op("dve", lambda e: e.reciprocal(out, in_), reads, writes)

    idb = cstb[:, C_ID:C_ID + 128]
    onesb = cstb[:, C_ONES:C_ONES + 128]
    bonesb = cstb[:, C_BONES:C_BONES + 128]
    rtb = cstb[:, C_RT:C_RT + 128]
    idf = cst[:, C_IDF:C_IDF + 128]

    def gs(g):
        return slice(g * GS, (g + 1) * GS)

    def rstd_from(bank, out, outt, inv_n, epscol, n=GS, extra_reads=()):
        ACT(out, pb[bank][:, :n], AF.Ln, [pbt[bank], cstt] + list(extra_reads), [outt], bias=CC(epscol), scale=inv_n)
        ACT(out, out, AF.Exp, [outt], [outt], scale=-0.5)

    COST = {"pe": 0.12, "act": 0.65, "dve": 0.6, "pool": 1.0, "sp": 0.0}

    def pipeline_gen(n, *gens):
        gens = [g_ if isinstance(g_, tuple) else (g_, i_) for i_, g_ in enumerate(gens)]
        maxlag = max(lag for _, lag in gens)
        for k in range(n + maxlag):
            chains = []
            for gfn, lag in gens:
                if 0 <= k - lag < n:
                    chains.append([gfn(k - lag), 0.0])
            teng = {e: 0.0 for e in ENGS}
            while chains:
                ch = min(chains, key=lambda c_: c_[1])
                P.log = []
                try:
                    next(ch[0])
                except StopIteration:
                    chains.remove(ch)
                for eng, cst_ in P.log:
                    start = max(ch[1], teng[eng])
                    teng[eng] = start + (COST[eng] if cst_ is None else cst_)
                    ch[1] = teng[eng] + 0.15
            P.log = None

    def pipeline(n, stage1, stage2):
        for k in range(n + 1):
            if k < n:
                stage1(k)
            if k >= 1:
                stage2(k - 1)

    wq = []

    def wspec(l):
        k = KINDS[l]
        s = []
        wkv = wd["wkv%d" % l].rearrange("(k p) n -> p k n", p=128)
        win = wd["win%d" % l].rearrange("(k p) n -> p k n", p=128)
        wout = wd["wout%d" % l].rearrange("(k p) n -> p k n", p=128)
        wup = wd["wup%d" % l].rearrange("(k p) n -> p k n", p=128)
        wdn = wd["wdn%d" % l].rearrange("(k p) n -> p k n", p=128)
        pre = [(wkv[:, :, 0:512], 8, 512), (wkv[:, :, 512:1024], 8, 512)]
        ncols = WIN_COLS[k]
        c0 = 0
        if k == 1:
            widths = [512, 512, 256, 512]
        else:
            widths = [512] * (ncols // 512)
        for w_ in widths:
            s.append((win[:, :, c0:c0 + w_], 8, w_))
            c0 += w_
        for ob in range(4):
            s.append((wout[:, :, ob * 256:(ob + 1) * 256], 12, 256))
        m = []
        for hb in range(8):
            m.append((wup[:, :, hb * 512:(hb + 1) * 512], 8, 512))
            m.append((wdn[:, hb * 4:(hb + 1) * 4, :], 4, 1024))
        return pre, s, m

    specs = [wspec(l) for l in layers]
    for i_, (pre_, s_, m_) in enumerate(specs):
        if i_ == 0:
            wq.extend(pre_)
        wq.extend(s_)
        wq.extend(m_[0:4])
        if i_ + 1 < len(specs):
            wq.extend(specs[i_ + 1][0])
        wq.extend(m_[4:])
    wstate = {"next_load": 0, "next_use": 0, "piece": 0}

    def w_prefetch(upto):
        while wstate["next_load"] < min(upto, len(wq)):
            i = wstate["next_load"]
            view, k, n = wq[i]
            slot = i % NSLOT
            dst = wslots[slot][:, 0:k * n].rearrange("p (k n) -> p k n", k=k)
            kk = max(1, STG // n)
            for k0 in range(0, k, kk):
                k1 = min(k, k0 + kk)
                j = wstate["piece"] % 2
                wstate["piece"] += 1
                stv = stage[j][:, 0:(k1 - k0) * n].rearrange("p (k n) -> p k n", k=k1 - k0)
                P.dma("sp", stv, view[:, k0:k1, :], reads=[], writes=[staget[j]])
                P.op("pool", lambda e, o=dst[:, k0:k1, :], i_=stv: e.tensor_copy(o, i_), [staget[j]], [wslott[slot]])
            wstate["next_load"] += 1

    def w_next(k, n, prefetch=True):
        i = wstate["next_use"]
        assert wq[i][1] == k and wq[i][2] == n, (i, wq[i][1:], k, n)
        w_prefetch(i + NSLOT if prefetch else i + 1)
        wstate["next_use"] += 1
        slot = i % NSLOT
        return wslots[slot][:, 0:k * n].rearrange("p (k n) -> p k n", k=k), wslott[slot]

    P.dma("sp", cst_[:], cst_d[:, NCB:NCONST], writes=[cstt])
    P.dma("sp", vec[:], vec_d, writes=[vect])
    sc = Scr()
    ctmp, ctmpt = sc.get([NCB], F32)
    P.dma("sp", ctmp, cst_d[:, 0:NCB], writes=[ctmpt])
    CP("dve", cstb[:], ctmp, [ctmpt], [cstbt])
    w_prefetch(NSLOT)
    xin, xint = [], []
    for i in range(2):
        a, t = sc.get([D], F32)
        xin.append(a)
        xint.append(t)
    for i in range(16):
        s_ = i % 2
        P.dma("sp", xin[s_], x_d[i * 128:(i + 1) * 128, :], writes=[xint[s_]])
        for half in range(2):
            b = nb()
            for c4 in range(4):
                c = half * 4 + c4
                TRN(pb[b][:, c4 * 128:(c4 + 1) * 128], xin[s_][:, c * 128:(c + 1) * 128], idf,
                    [xint[s_], cstt], [pbt[b]], inc=(c4 == 3))
            g = i // 4
            wr = [xTt[half * 4 + c4][g] for c4 in range(4)]
            CP("dve" if half == 0 else "act", xT[:, half * 4:half * 4 + 4, i * 128:(i + 1) * 128],
               pb[b][:].rearrange("p (c t) -> p c t", c=4), [pbt[b]], wr)
    sc.close()

    def prenorm(gname):
        sc = Scr()
        sqs = [sc.get([8, GS], BF16) for _ in range(2)]
        rs = [sc.get([GS], F32) for _ in range(2)]
        banks = {}

        def s1(g):
            sq, sqt = sqs[g % 2]
            for c in range(8):
                ACT(sq[:, c, :], xT[:, c, gs(g)], AF.Square, [xTt[c][g]], [sqt])
            b = nb()
            banks[g] = b
            for c in range(8):
                MM(pb[b][:], onesb, sq[:, c, :], c == 0, c == 7, [sqt, cstbt], [pbt[b]])

        def s2(g):
            r, rt = rs[g % 2]
            rstd_from(banks[g], r, rt, 1.0 / D, C_EPSR)
            for c in range(8):
                STT(hT[:, c, gs(g)], xT[:, c, gs(g)], V(gname, c), r, ALU.mult, ALU.mult,
                    [xTt[c][g], rt, vect], [hTt[g]])
        pipeline(NG, s1, s2)
        sc.close()

    def proj_fm(wt, wtt, c0, g, pool=None):
        b = nb(pool)
        for kc in range(8):
            MM(pb[b][:], wt[:, kc, c0:c0 + 128], hT[:, kc, gs(g)], kc == 0, kc == 7, [wtt, hTt[g]], [pbt[b]])
        return b

    def proj_gen(wt, wtt, c0, g, pool):
        b = nb(pool)
        for kc in range(8):
            MM(pb[b][:], wt[:, kc, c0:c0 + 128], hT[:, kc, gs(g)], kc == 0, kc == 7, [wtt, hTt[g]], [pbt[b]])
            if kc == 3:
                yield
        return b

    def mt_grp(c, g):
        return mixt[c][4 * g:4 * g + 4]

    def xa_prep_p1(l, sc):
        mm_, mmt = sc.get([2, D], F32)
        junk, junkt = sc.get([D], BF16)
        memn, memnt = sc.get([8, 256], BF16)
        ksq, ksqt = sc.get([4, 256], BF16)
        krs, krst = sc.get([4, 256], F32)
        ssq = small[:, 0:2]
        for mt in range(2):
            P.dma("sp", mm_[:, mt, :], mem_d[mt * 128:(mt + 1) * 128, :], writes=[mmt])
        for mt in range(2):
            ACT(junk, mm_[:, mt, :], AF.Square, [mmt], [junkt, smallt], accum=ssq[:, mt:mt + 1])
        ACT(ssq, ssq, AF.Ln, [smallt, cstt], [smallt], bias=CC(C_EPSR), scale=1.0 / D)
        ACT(ssq, ssq, AF.Exp, [smallt], [smallt], scale=-0.5)
        for mt in range(2):
            TS("dve", mm_[:, mt, :], mm_[:, mt, :], ssq[:, mt:mt + 1], None, ALU.mult, None, [mmt, smallt], [mmt])
        gkq = small[:, 2:3]
        TS("dve", gkq, V("xak%d" % l), V("xaq%d" % l), 128.0 ** -0.5, ALU.mult, ALU.mult, [vect], [smallt])
        return (mm_, mmt, memn, memnt, ksq, ksqt, krs, krst, gkq)

    def xa_prep_p2a(l, bufs):
        mm_, mmt, memn, memnt, ksq, ksqt, krs, krst, gkq = bufs
        for c in range(8):
            b = nb()
            for mt in range(2):
                TRN(pb[b][:, mt * 128:(mt + 1) * 128], mm_[:, mt, c * 128:(c + 1) * 128], idf, [mmt, cstt], [pbt[b]],
                    inc=(mt == 1))
            yield
            TS("dve", memn[:, c, :], pb[b][:, 0:256], V("nmem%d" % l, c), None, ALU.mult, None, [pbt[b], vect], [memnt])
            yield

    def xa_prep_p2(l, bufs):
        mm_, mmt, memn, memnt, ksq, ksqt, krs, krst, gkq = bufs
        wk, wkt = w_next(8, 512)
        kb = []
        for h in range(4):
            if h % 2 == 0:
                b = nb()
                kb.append(b)
            for kc in range(8):
                MM(pb[b][:, (h % 2) * 256:(h % 2 + 1) * 256], wk[:, kc, h * 128:(h + 1) * 128], memn[:, kc, :],
                   kc == 0, kc == 7, [wkt, memnt], [pbt[b]])
        for hp in range(2):
            ACT(ksq[:, 2 * hp:2 * hp + 2, :], pb[kb[hp]][:].rearrange("p (a b) -> p a b", a=2), AF.Square,
                [pbt[kb[hp]]], [ksqt])
        for hp in range(2):
            b = nb()
            MM(pb[b][:], onesb, ksq[:, 2 * hp:2 * hp + 2, :], True, True, [ksqt, cstbt], [pbt[b]])
            kr = krs[:, 2 * hp:2 * hp + 2, :]
            ACT(kr, pb[b][:].rearrange("p (a b) -> p a b", a=2), AF.Ln, [pbt[b], cstt], [krst],
                bias=CC(C_EPSR), scale=1.0 / 128)
            ACT(kr, kr, AF.Exp, [krst], [krst], scale=-0.5)
            STT(kxa[:, 2 * hp:2 * hp + 2, :], pb[kb[hp]][:].rearrange("p (a b) -> p a b", a=2), gkq, kr,
                ALU.mult, ALU.mult, [pbt[kb[hp]], krst, smallt], [kxat])
        wv, wvt = w_next(8, 512)
        for mt in range(2):
            b = nb()
            for kc in range(8):
                MM(pb[b][:], memn[:, kc, mt * 128:(mt + 1) * 128], wv[:, kc, :], kc == 0, kc == 7,
                   [wvt, memnt], [pbt[b]])
            CP("act", vxa[:, mt, :], pb[b][:], [pbt[b]], [vxat])

    def xa_attend(sc, wt, wtt):
        sqs = [sc.get([GS], BF16) for _ in range(2)]
        rss = [sc.get([GS], F32) for _ in range(2)]
        ee = [sc.get([2, 256], BF16) for _ in range(2)]
        rds = [sc.get([256], F32) for _ in range(2)]
        st = {}

        def q1(k):
            h, g = divmod(k, NG)
            sq, sqt = sqs[k % 2]
            bq = proj_fm(wt, wtt, h * 128, g)
            ACT(sq, pb[bq][:], AF.Square, [pbt[bq]], [sqt])
            bs = nb()
            MM(pb[bs][:], onesb, sq, True, True, [sqt, cstbt], [pbt[bs]])
            st[k] = (bq, bs)

        def q2(k):
            h, g = divmod(k, NG)
            bq, bs = st[k]
            rs, rst = rss[k % 2]
            rstd_from(bs, rs, rst, 1.0 / 128, C_EPSR)
            TT("dve", mixT[:, 8 + h, gs(g)], pb[bq][:], rs, ALU.mult, [pbt[bq], rst], mt_grp(8 + h, g))
        pipeline(16, q1, q2)

        def a1(k):
            h, r_ = divmod(k, 8)
            cols = slice(r_ * 256, (r_ + 1) * 256)
            qt = mixt[8 + h][2 * r_:2 * r_ + 2]
            e, et = ee[k % 2]
            b = nb()
            for mt in range(2):
                MM(pb[b][:, mt * 256:(mt + 1) * 256], kxa[:, h, mt * 128:(mt + 1) * 128], mixT[:, 8 + h, cols], True, True,
                   [kxat] + qt, [pbt[b]], inc=(mt == 1))
            ACT(e, pb[b][:].rearrange("p (a b) -> p a b", a=2), AF.Exp, [pbt[b]], [et])

        def a2(k):
            h, r_ = divmod(k, 8)
            cols = slice(r_ * 256, (r_ + 1) * 256)
            qt = mixt[8 + h][2 * r_:2 * r_ + 2]
            e, et = ee[k % 2]
            rd, rdt = rds[k % 2]
            bn = nb()
            for mt in range(2):
                MM(pb[bn][:, 0:256], vxa[:, mt, h * 128:(h + 1) * 128], e[:, mt, :], mt == 0, mt == 1, [vxat, et], [pbt[bn]],
                   inc=False)
            for mt in range(2):
                MM(pb[bn][:, 256:512], onesb, e[:, mt, :], mt == 0, mt == 1, [cstbt, et], [pbt[bn]], inc=(mt == 1))
            ACT(rd, pb[bn][:, 256:512], AF.Ln, [pbt[bn]], [rdt])
            ACT(rd, rd, AF.Exp, [rdt], [rdt], scale=-1.0)
            TT("dve", mixT[:, 8 + h, cols], pb[bn][:, 0:256], rd, ALU.mult, [pbt[bn], rdt], qt)
        pipeline(32, a1, a2)

    def out_proj(l):
        for ob in range(4):
            wt, wtt = w_next(12, 256)
            for oo in range(2):
                o = ob * 2 + oo
                for g in range(NG):
                    b = nb()
                    for kc in range(12):
                        MM(pb[b][:], wt[:, kc, oo * 128:(oo + 1) * 128], mixT[:, kc, gs(g)], kc == 0, kc == 11,
                           [wtt] + mt_grp(kc, g), [pbt[b]])
                    TT("dve", xT[:, o, gs(g)], pb[b][:], xT[:, o, gs(g)], ALU.add, [pbt[b], xTt[o][g]], [xTt[o][g]])

    def mlp(l, next_l=None):
        prenorm("nmlp%d" % l)
        sc = Scr()
        rl = [sc.get([GS], F32) for _ in range(3)]
        xbufs = xa_prep_p1(next_l, sc) if next_l is not None else None
        xgen = xa_prep_p2a(next_l, xbufs) if next_l is not None else iter(())

        def xstep():
            try:
                next(xgen)
            except StopIteration:
                pass
        u = 0
        for hb in range(8):
            if hb == 2 and xbufs is not None:
                for _ in xgen:
                    pass
                xa_prep_p2(next_l, xbufs)
            wu, wut = w_next(8, 512)
            ab = (hb % 2) * 4
            for j in range(4):
                for g in range(NG):
                    b = proj_fm(wu, wut, j * 128, g)
                    r, rt = rl[u % 3]
                    u += 1
                    ACT(r, pb[b][:], AF.Relu, [pbt[b]], [rt])
                    TT("pool", mixT[:, ab + j, gs(g)], r, r, ALU.mult, [rt], mt_grp(ab + j, g))
                    if hb >= 1:
                        xstep()
            wdt, wdtt = w_next(4, 1024)
            for o in range(8):
                for g in range(NG):
                    b = nb()
                    for j in range(4):
                        MM(pb[b][:], wdt[:, j, o * 128:(o + 1) * 128], mixT[:, ab + j, gs(g)], j == 0, j == 3,
                           [wdtt] + mt_grp(ab + j, g), [pbt[b]])
                    TT("dve", xT[:, o, gs(g)], pb[b][:], xT[:, o, gs(g)], ALU.add, [pbt[b], xTt[o][g]], [xTt[o][g]])
        sc.close()

    def lb_prep(l):
        e_ = small[:, 24:56].rearrange("p (l c) -> p l c", l=4)
        for ll in range(4):
            ACT(e_[:, ll, :], vec[:, vidx["lbl%d" % ll]:vidx["lbl%d" % ll] + 8], AF.Exp, [vect], [smallt])
        tot = small[:, 56:64]
        TT("dve", tot, e_[:, 0, :], e_[:, 1, :], ALU.add, [smallt], [smallt])
        TT("dve", tot, tot, e_[:, 2, :], ALU.add, [smallt], [smallt])
        TT("dve", tot, tot, e_[:, 3, :], ALU.add, [smallt], [smallt])
        RECIP(tot, tot, [smallt], [smallt])
        lb = small[:, 8:16]
        P.op("dve", lambda e: e.memset(lb, 0.0), [], [smallt])
        for ll in range(1, l + 1):
            TT("dve", lb, lb, e_[:, ll, :], ALU.add, [smallt], [smallt])
        TT("dve", lb, lb, tot, ALU.mult, [smallt], [smallt])
        oml = small[:, 16:24]
        TS("dve", oml, lb, -1.0, 1.0, ALU.mult, ALU.add, [smallt], [smallt])

    def mixer_hgrn(l):
        idx = KINDS[:l].count(0)
        lb_prep(l)
        sc = Scr()
        Fs = [[sc.get([GS], F32) for _ in range(4)] for _ in range(2)]
        qes = [sc.get([GS], BF16) for _ in range(3)]
        kebs = [sc.get([GS], BF16) for _ in range(3)]
        sgts = [sc.get([GS], BF16) for _ in range(3)]
        xflat = mixT[:, 8:12, :].rearrange("p c t -> p (c t)").bitcast(F32)
        xinh = []
        for c_ in range(8, 12):
            for m_ in mixt[c_]:
                xinh.extend(([m_.w] if m_.w else []) + m_.r)
        xst = {"off": 0, "t": []}

        def xget(free_shape, dtype):
            n_ = int(np.prod(free_shape))
            words = n_ if dtype == F32 else (n_ + 1) // 2
            a_ = xflat[:, xst["off"]:xst["off"] + words]
            xst["off"] += words
            assert xst["off"] <= 4096
            if dtype != F32:
                a_ = a_.bitcast(dtype)
            if len(free_shape) == 2:
                a_ = a_.rearrange("p (a b) -> p a b", a=free_shape[0])
            t_ = Trk()
            t_.r = list(xinh)
            xst["t"].append(t_)
            return a_, t_
        Sall, _ = xget([9, 128], F32)
        Sallt = [Trk() for _ in range(9)]
        for t_ in Sallt:
            t_.r = list(xinh)
            xst["t"].append(t_)
        k2ts = [xget([4, 128], BF16) for _ in range(3)]
        vtks = [xget([4, 128], BF16) for _ in range(3)]
        ees = [xget([16], F32) for _ in range(3)]
        R1, R1t = xget([GS], F32)
        am, amt = xget([GS], BF16)
        sta, stat = xget([8, 128], BF16)
        stats = [stat] + [Trk() for _ in range(7)]
        for t_ in stats[1:]:
            t_.r = list(xinh)
            xst["t"].append(t_)
        chm = cst[:, C_CHM:C_CHM + GS]
        maskA = cstb[:, C_MASKA:C_MASKA + GS]
        LN_MIN = math.log(1e-20)
        wts = {}
        banks = {}

        def sA1(k):
            h, g = divmod(k, NG)
            if g == 0:
                wts[h] = w_next(8, 512, prefetch=(h == 0))
            wt, wtt = wts[h]
            (F1, F1t), (F2, F2t), (F3, F3t), (F4, F4t) = Fs[k % 2]
            lbc = small[:, 8 + h:9 + h]
            bf = yield from proj_gen(wt, wtt, 128, g, "A1")
            yield
            ACT(F1, pb[bf][:], AF.Exp, [pbt[bf]], [F1t], scale=-1.0)
            ACT(F2, F1, AF.Ln, [F1t], [F2t], bias=1.0)
            ACT(F3, F1, AF.Ln, [F1t, smallt], [F3t], bias=1.0, scale=lbc)
            yield
            STT(F3, F2, -1.0, F3, ALU.mult, ALU.add, [F2t, F3t], [F3t])
            TS("dve", F3, F3, LN_MIN, None, ALU.max, None, [F3t], [F3t])
            STT(F1, pb[bf][:], -1.0, F2, ALU.mult, ALU.subtract, [pbt[bf], F2t], [F1t])
            yield
            ACT(F1, F1, AF.Exp, [F1t], [F1t])

        def sA1g(k):
            h, g = divmod(k, NG)
            wt, wtt = wts[h]
            (F1, F1t), (F2, F2t), (F3, F3t), (F4, F4t) = Fs[k % 2]
            sgt_, sgtt = sgts[k % 3]
            bg = yield from proj_gen(wt, wtt, 384, g, "A1g")
            yield
            ACT(F4, pb[bg][:], AF.Exp, [pbt[bg]], [F4t], scale=-1.0)
            yield
            ACT(F4, F4, AF.Ln, [F4t], [F4t], bias=1.0)
            yield
            ACT(F4, F4, AF.Exp, [F4t], [F4t], scale=-1.0)
            yield
            STT(sgt_, pb[bg][:], V("aon%d" % idx, h), F4, ALU.mult, ALU.mult, [pbt[bg], F4t, vect], [sgtt])

        def sA2(k):
            h, g = divmod(k, NG)
            wt, wtt = wts[h]
            (F1, F1t), (F2, F2t), (F3, F3t), (F4, F4t) = Fs[k % 2]
            qe, qet = qes[k % 3]
            keb, kebt = kebs[k % 3]
            k2t, k2tt = k2ts[k % 3]
            vtk, vtkt = vtks[k % 3]
            ee, eet = ees[k % 3]
            omc = small[:, 16 + h:17 + h]
            P.op("dve", lambda e: e.tensor_tensor_scan(F2, chm, F3, 0.0, ALU.mult, ALU.add), [cstt, F3t], [F2t])
            b3 = F2.rearrange("p (c t) -> p c t", c=8)
            bp3 = F3.rearrange("p (c t) -> p c t", c=8)
            TT("dve", bp3, b3, b3[:, :, 31:32].to_broadcast([128, 8, 64]), ALU.subtract, [F2t], [F3t])
            yield
            bq = yield from proj_gen(wt, wtt, 0, g, "A2")
            yield
            ACT(F4, F3, AF.Exp, [F3t], [F4t])
            ACT(ee[:, 0:8], b3[:, :, 63], AF.Exp, [F2t], [eet])
            ACT(ee[:, 8:16], b3[:, :, 31], AF.Exp, [F2t], [eet])
            ACT(F2, F3, AF.Exp, [F3t], [F2t], scale=-1.0)
            yield
            bv = nb("A2")
            for i in range(4):
                for kc in range(8):
                    MM(pb[bv][:, i * 128:(i + 1) * 128], hT[:, kc, g * GS + i * 128:g * GS + (i + 1) * 128],
                       wt[:, kc, 256:384], kc == 0, kc == 7, [wtt, hTt[g]], [pbt[bv]], inc=(kc == 7 and i == 3))
                yield
            TT("dve", qe, pb[bq][:], F4, ALU.mult, [pbt[bq], F4t], [qet])
            STT(keb, F1, omc, F2, ALU.mult, ALU.mult, [F1t, F2t, smallt], [kebt])
            yield
            eb3 = F4.rearrange("p (c t) -> p c t", c=8)
            TT("pool", F3.rearrange("p (c t) -> p c t", c=8), keb.rearrange("p (c t) -> p c t", c=8),
               eb3[:, :, 63:64].to_broadcast([128, 8, 64]), ALU.mult, [kebt, F4t], [F3t])
            CP("act", vtk, pb[bv][:].rearrange("p (a b) -> p a b", a=4), [pbt[bv]], [vtkt])
            yield
            bt = nb("A2")
            for i in range(4):
                TRN(pb[bt][:, i * 128:(i + 1) * 128], F3[:, i * 128:(i + 1) * 128], idf, [F3t, cstt], [pbt[bt]],
                    inc=(i == 3))
            yield
            CP("act", k2t, pb[bt][:].rearrange("p (a b) -> p a b", a=4), [pbt[bt]], [k2tt])
            if g == NG - 1:
                w_prefetch(wstate["next_use"] + 1)

        def sB(k):
            h, g = divmod(k, NG)
            qe, qet = qes[k % 3]
            keb, kebt = kebs[k % 3]
            sgt_, sgtt = sgts[k % 3]
            k2t, k2tt = k2ts[k % 3]
            vtk, vtkt = vtks[k % 3]
            ee, eet = ees[k % 3]
            if g == 0:
                P.op("dve", lambda e: e.memset(Sall[:, 0, :], 0.0), [], [Sallt[0]])
            else:
                CP("dve", Sall[:, 0, :], Sall[:, 8, :], [Sallt[8]], [Sallt[0]])
            bk = [nb("B3"), nb("B3")]
            for par in range(2):
                for i in range(4):
                    MM(pb[bk[par]][:, i * 128:(i + 1) * 128], k2t[par * 64:(par + 1) * 64, i, :],
                       vtk[par * 64:(par + 1) * 64, i, :], True, True, [k2tt, vtkt], [pbt[bk[par]]], inc=(i == 3))
            yield
            ba = nb("B3")
            for i in range(4):
                MM(pb[ba][:, i * 128:(i + 1) * 128], keb[:, i * 128:(i + 1) * 128], qe[:, i * 128:(i + 1) * 128],
                   True, True, [kebt, qet], [pbt[ba]], inc=(i == 3))
            yield
            for cc in range(8):
                ACT(sta[:, cc, :], Sall[:, cc, :], AF.Copy, [Sallt[cc], eet], [stats[cc]], scale=ee[:, 8 + cc:9 + cc])
                STT(Sall[:, cc + 1, :], Sall[:, cc, :], ee[:, cc:cc + 1],
                    pb[bk[cc % 2]][:, (cc // 2) * 128:(cc // 2 + 1) * 128],
                    ALU.mult, ALU.add, [Sallt[cc], eet, pbt[bk[cc % 2]]], [Sallt[cc + 1]])
                if cc == 1:
                    TT("dve", am, pb[ba][:], maskA, ALU.mult, [pbt[ba], cstbt], [amt])
                yield
            bo = nb("B3")
            for i in range(4):
                MM(pb[bo][:, i * 128:(i + 1) * 128], vtk[:, i, :], am[:, i * 128:(i + 1) * 128], True, False,
                   [vtkt, amt], [pbt[bo]], inc=False)
                for hh in range(2):
                    cc = 2 * i + hh
                    MM(pb[bo][:, cc * 64:(cc + 1) * 64], sta[:, cc, :], qe[:, cc * 64:(cc + 1) * 64], False, hh == 1,
                       [stats[cc], qet], [pbt[bo]], inc=(hh == 1 and i == 3))
            yield
            ACT(am, pb[bo][:], AF.Square, [pbt[bo]], [amt])
            yield
            bs = nb("B3")
            MM(pb[bs][:], onesb, am, True, True, [amt, cstbt], [pbt[bs]])
            yield
            rstd_from(bs, R1, R1t, 1.0 / 128, C_EPSR)
            yield
            TT("dve", R1, pb[bo][:], R1, ALU.mult, [pbt[bo], R1t], [R1t])
            TT("dve", mixT[:, h, gs(g)], R1, sgt_, ALU.mult, [R1t, sgtt], mt_grp(h, g))

        pipeline_gen(32, (sA1, 0), (sA1g, 0), (sA2, 1), (sB, 2))
        for t_ in xst["t"]:
            for c_ in range(8, 12):
                for m_ in mixt[c_]:
                    m_.r.extend(([t_.w] if t_.w else []) + t_.r)
        wt, wtt = w_next(8, 512)
        sc.release(0)
        xa_attend(sc, wt, wtt)
        sc.close()

    def mixer_swa(l):
        sc = Scr()
        kT = [(mixT[:, 8, :], Trk()), (mixT[:, 9, :], Trk())]
        vtk, vtkt = mixT[:, 10, :].rearrange("p (a b) -> p a b", a=16), Trk()
        cosT, cost = mixT[:, 11, :], Trk()
        for t_ in (kT[0][1], kT[1][1], vtkt, cost):
            for c_ in range(8, 12):
                for m_ in mixt[c_]:
                    t_.r.extend(([m_.w] if m_.w else []) + m_.r)
        sinT, sint = sc.get([T], BF16)
        esk = small[:, 8:16]
        ACT(esk, vec[:, vidx["sink"]:vidx["sink"] + 8], AF.Exp, [vect], [smallt])
        sc_mark = sc.off
        pi_, pit = sc.get([GS], I32)
        a1, a1t = sc.get([GS], F32)
        a2, a2t = sc.get([GS], F32)
        a3, a3t = sc.get([GS], F32)
        MAGIC = 12582912.0
        C1 = 6.28125
        C2 = TWO_PI - 6.28125
        for g in range(NG):
            P.dma("sp", pi_, pos_d[0:1, gs(g)].partition_broadcast(128), writes=[pit])
            CP("dve", a1, pi_, [pit], [a1t])
            TS("dve", a1, a1, CC(C_INVF), None, ALU.mult, None, [a1t, cstt], [a1t])
            for which, dst, dstt in ((0, sinT, sint), (1, cosT, cost)):
                if which == 1:
                    TS("dve", a1, a1, CC(C_HALFPI), None, ALU.add, None, [a1t, cstt], [a1t])
                TS("dve", a2, a1, 1.0 / TWO_PI, MAGIC, ALU.mult, ALU.add, [a1t], [a2t])
                TS("dve", a2, a2, -MAGIC, None, ALU.add, None, [a2t], [a2t])
                STT(a3, a2, -C1, a1, ALU.mult, ALU.add, [a2t, a1t], [a3t])
                STT(a3, a2, -C2, a3, ALU.mult, ALU.add, [a2t, a3t], [a3t])
                TS("dve", a3, a3, math.pi, -math.pi, ALU.min, ALU.max, [a3t], [a3t])
                ACT(dst[:, gs(g)], a3, AF.Sin, [a3t], [dstt])
        sc.release(sc_mark)
        sqs = [sc.get([GS], BF16) for _ in range(2)]
        qgs = [sc.get([GS], BF16) for _ in range(2)]
        rss = [sc.get([GS], F32) for _ in range(2)]
        t1s = [sc.get([GS], F32) for _ in range(2)]
        t2s = [sc.get([GS], F32) for _ in range(2)]
        units = []
        for tix in range(2):
            for cj in range(4):
                for g in range(NG):
                    units.append(("q", tix, cj, g))
        for gk in range(2):
            for g in range(NG):
                units.append(("k", gk, 0, g))
        wcache = {}
        st = {}

        def get_w(key, k_, n_):
            if key not in wcache:
                wcache[key] = w_next(k_, n_)
            return wcache[key]

        def r1(k):
            kind_, a_, cj, g = units[k]
            if kind_ == "q":
                wt, wtt = get_w(("q", a_), 8, 512)
                b = proj_fm(wt, wtt, cj * 128, g)
                gcol = V("bq")
            else:
                wt, wtt = get_w("kv", 8, 256)
                b = nb()
                for rep in range(2):
                    for kc in range(8):
                        MM(pb[b][rep * 64:(rep + 1) * 64, :], wt[:, kc, a_ * 64:(a_ + 1) * 64], hT[:, kc, gs(g)],
                           kc == 0, kc == 7, [wtt, hTt[g]], [pbt[b]], inc=(kc == 7 and rep == 1))
                gcol = V("bk")
            sq, sqt = sqs[k % 2]
            qg, qgt = qgs[k % 2]
            ACT(sq, pb[b][:], AF.Square, [pbt[b]], [sqt])
            ACT(qg, pb[b][:], AF.Copy, [pbt[b], vect], [qgt], scale=gcol)
            bm = nb()
            MM(pb[bm][:], bonesb, sq, True, True, [sqt, cstbt], [pbt[bm]])
            br = nb()
            MM(pb[br][:], rtb, qg, True, True, [qgt, cstbt], [pbt[br]])
            st[k] = (bm, br)

        def r2(k):
            kind_, a_, cj, g = units[k]
            bm, br = st[k]
            qg, qgt = qgs[k % 2]
            rs, rst = rss[k % 2]
            t1, t1t = t1s[k % 2]
            t2, t2t = t2s[k % 2]
            if kind_ == "q":
                c = a_ * 4 + cj
                dst, dstt_list = mixT[:, c, gs(g)], mt_grp(c, g)
            else:
                dst, dstt_list = kT[a_][0][:, gs(g)], [kT[a_][1]]
            rstd_from(bm, rs, rst, 1.0 / 64, C_EPSR)
            TT("pool", t1, qg, cosT[:, gs(g)], ALU.mult, [qgt, cost], [t1t])
            TT("dve", t2, pb[br][:], sinT[:, gs(g)], ALU.mult, [pbt[br], sint], [t2t])
            TT("dve", t1, t1, t2, ALU.add, [t1t, t2t], [t1t])
            TT("dve", dst, t1, rs, ALU.mult, [t1t, rst], dstt_list)
        pipeline(len(units), r1, r2)
        wt, wtt = get_w("kv", 8, 256)
        for i4 in range(4):
            b = nb()
            for ii in range(4):
                i = i4 * 4 + ii
                for kc in range(8):
                    MM(pb[b][:, ii * 128:(ii + 1) * 128], hT[:, kc, i * 128:(i + 1) * 128], wt[:, kc, 128:256],
                       kc == 0, kc == 7, [wtt, hTt[i4]], [pbt[b]], inc=(kc == 7 and ii == 3))
            CP("act", vtk[:, i4 * 4:i4 * 4 + 4, :], pb[b][:].rearrange("p (a b) -> p a b", a=4), [pbt[b]], [vtkt])
        sc.release(sc_mark)
        EE = []
        for _ in range(2):
            e_, t0_ = sc.get([2, 2, 128], BF16)
            t1_ = Trk()
            t1_.r = list(t0_.r)
            sc.t.append(t1_)
            EE.append((e_, (t0_, t1_)))
        dn_ = [sc.get([128], F32) for _ in range(2)]
        swam = cstb[:, C_SWAM:C_SWAM + 256].rearrange("p (a b) -> p a b", a=2)

        def c1(u):
            c, n = divmod(u, 16)
            gk = c // 4
            kTa, kTt = kT[gk]
            kbs = [1] if n == 0 else [0, 1]
            e, et = EE[u % 2]
            q_t = [mixt[c][n]]
            for par in range(2):
                b = nb("A")
                for kb_ in kbs:
                    nbk = n - 1 + kb_
                    MM(pb[b][:, kb_ * 128:(kb_ + 1) * 128], kTa[par * 64:(par + 1) * 64, nbk * 128:(nbk + 1) * 128],
                       mixT[par * 64:(par + 1) * 64, c, n * 128:(n + 1) * 128], True, True, [kTt] + q_t, [pbt[b]],
                       inc=(kb_ == 1))
                lo = kbs[0]
                ACT(e[:, par, lo:2, :], pb[b][:, lo * 128:256].rearrange("p (a b) -> p a b", b=128), AF.Exp,
                    [pbt[b]], [et[par]], scale=0.125)
                TT("pool" if par == 0 else "dve", e[:, par, lo:2, :], e[:, par, lo:2, :], swam[:, lo:2, :], ALU.mult,
                   [et[par], cstbt], [et[par]])
                yield

        def c2(u):
            c, n = divmod(u, 16)
            gk = c // 4
            kbs = [1] if n == 0 else [0, 1]
            e, et = EE[u % 2]
            dn, dnt = dn_[u % 2]
            bn = nb("B")
            for par in range(2):
                for which in range(2):
                    for kb_ in kbs:
                        nbk = n - 1 + kb_
                        lhs = vtk[:, nbk, gk * 64:(gk + 1) * 64] if which == 0 else onesb[:, 0:64]
                        MM(pb[bn][par * 64:(par + 1) * 64, which * 128:(which + 1) * 128], lhs, e[:, par, kb_, :],
                           kb_ == kbs[0], kb_ == 1, [vtkt, cstbt, et[par]], [pbt[bn]],
                           inc=(kb_ == 1 and which == 1 and par == 1))
            yield
            ACT(dn, pb[bn][:, 128:256], AF.Ln, [pbt[bn], smallt], [dnt], bias=esk[:, c:c + 1])
            ACT(dn, dn, AF.Exp, [dnt], [dnt], scale=-1.0)
            yield
            TT("dve", mixT[:, c, n * 128:(n + 1) * 128], pb[bn][:, 0:128], dn, ALU.mult, [pbt[bn], dnt], [mixt[c][n]])
        pipeline(128, lambda k_: list(c1(k_)), lambda k_: list(c2(k_)))
        for t_ in (kT[0][1], kT[1][1], vtkt, cost):
            for c_ in range(8, 12):
                for m_ in mixt[c_]:
                    m_.r.extend(([t_.w] if t_.w else []) + t_.r)
        wt, wtt = w_next(8, 512)
        sc.release(0)
        xa_attend(sc, wt, wtt)
        sc.close()

    def mixer_conv(l):
        sc = Scr()
        UW = 30 + T
        uT = [sc.get([UW], BF16) for _ in range(2)]
        dgs = [sc.get([31, 128], BF16) for _ in range(2)]
        sg = [sc.get([GS], BF16) for _ in range(2)]
        cwv = vec[:, vidx["cw"]:vidx["cw"] + 8 * 31].rearrange("p (c w) -> p c w", c=8)
        for u_, ut_ in uT:
            P.op("pool", lambda e, u_=u_: e.memset(u_[:, 0:30], 0.0), [], [ut_])
        su = 0
        wts = {}

        def build_dg(c):
            dg, dgt = dgs[c % 2]
            for w_ in range(31):
                ACT(dg[:, w_, :], idb, AF.Copy, [cstbt, vect], [dgt], scale=cwv[:, c, w_:w_ + 1])

        def glu(c):
            blk, j = divmod(c, 2)
            if blk not in wts:
                wts[blk] = w_next(8, 512)
            wt, wtt = wts[blk]
            u_, ut_ = uT[c % 2]
            for g in range(NG):
                ba = proj_fm(wt, wtt, j * 128, g)
                bg = proj_fm(wt, wtt, 256 + j * 128, g)
                s_, st_ = sg[(c * NG + g) % 2]
                ACT(s_, pb[bg][:], AF.Sigmoid, [pbt[bg]], [st_])
                TT("dve", u_[:, 30 + g * GS:30 + (g + 1) * GS], pb[ba][:], s_, ALU.mult, [pbt[ba], st_], [ut_])

        def conv(c):
            u_, ut_ = uT[c % 2]
            dg, dgt = dgs[c % 2]
            for g in range(NG):
                b = nb()
                for w_ in range(31):
                    MM(pb[b][:], dg[:, w_, :], u_[:, g * GS + w_:g * GS + w_ + GS], w_ == 0, w_ == 30,
                       [dgt, ut_], [pbt[b]])
                ACT(mixT[:, c, gs(g)], pb[b][:], AF.Identity, [pbt[b], vect], mt_grp(c, g), bias=V("cb", c))

        for c in range(9):
            if c < 8:
                build_dg(c)
                glu(c)
            if c >= 1:
                conv(c - 1)
        sc.release(0)
        ysqs = [sc.get([GS], BF16) for _ in range(2)]
        mus = [sc.get([GS], F32) for _ in range(2)]
        m2s = [sc.get([GS], F32) for _ in range(2)]
        rss = [sc.get([GS], F32) for _ in range(2)]
        tt_ = [sc.get([GS], F32) for _ in range(2)]
        st = {}

        def l1(g):
            ysq, ysqt = ysqs[g % 2]
            b1 = nb()
            for c in range(8):
                MM(pb[b1][:], onesb, mixT[:, c, gs(g)], c == 0, c == 7, [cstbt] + mt_grp(c, g), [pbt[b1]])
            b2 = nb()
            for c in range(8):
                ACT(ysq, mixT[:, c, gs(g)], AF.Square, mt_grp(c, g), [ysqt])
                MM(pb[b2][:], onesb, ysq, c == 0, c == 7, [cstbt, ysqt], [pbt[b2]], inc=True)
            st[g] = (b1, b2)

        def l2(g):
            b1, b2 = st[g]
            mu, mut = mus[g % 2]
            m2, m2t = m2s[g % 2]
            rs, rst = rss[g % 2]
            ACT(mu, pb[b1][:], AF.Copy, [pbt[b1]], [mut], scale=1.0 / D)
            TT("pool", m2, mu, mu, ALU.mult, [mut], [m2t])
            STT(m2, pb[b2][:], 1.0 / D, m2, ALU.mult, ALU.subtract, [pbt[b2], m2t], [m2t])
            ACT(rs, m2, AF.Ln, [m2t, cstt], [rst], bias=CC(C_EPSL), scale=1.0)
            ACT(rs, rs, AF.Exp, [rst], [rst], scale=-0.5)
            for c in range(8):
                t_, ttt = tt_[c % 2]
                TT("dve", t_, mixT[:, c, gs(g)], mu, ALU.subtract, mt_grp(c, g) + [mut], [ttt])
                TT("dve", t_, t_, rs, ALU.mult, [ttt, rst], [ttt])
                ACT(mixT[:, c, gs(g)], t_, AF.Silu, [ttt, vect], mt_grp(c, g), bias=V("lnb", c), scale=V("lng", c))
        pipeline(NG, l1, l2)
        wt, wtt = w_next(8, 512)
        sc.release(0)
        xa_attend(sc, wt, wtt)
        sc.close()

    for li, l in enumerate(layers):
        if li == 0:
            sc0 = Scr()
            xb0 = xa_prep_p1(l, sc0)
            for _ in xa_prep_p2a(l, xb0):
                pass
            xa_prep_p2(l, xb0)
            sc0.close()
        prenorm("nmix%d" % l)
        kind = KINDS[l]
        if kind == 0:
            mixer_hgrn(l)
        elif kind == 1:
            mixer_swa(l)
        else:
            mixer_conv(l)
        out_proj(l)
        mlp(l, layers[li + 1] if li + 1 < len(layers) else None)

    sc = Scr()
    yo = [sc.get([D], F32) for _ in range(2)]
    out_toks = []
    for i in range(16):
        y_, yt_ = yo[i % 2]
        g = i // 4
        for half in range(2):
            b = nb()
            for c4 in range(4):
                c = half * 4 + c4
                TRN(pb[b][:, c4 * 128:(c4 + 1) * 128], xT[:, c, i * 128:(i + 1) * 128], idf, [xTt[c][g], cstt], [pbt[b]],
                    inc=(c4 == 3))
            CP("dve" if half == 0 else "act", y_[:, half * 512:(half + 1) * 512], pb[b][:], [pbt[b]], [yt_])
        out_toks.append(P.dma("sp", y_d[i * 128:(i + 1) * 128, :], y_, reads=[yt_]))
    sc.close()
    P.wait_tok("sp", out_toks)
    P.emit()
    return nc


_CACHE = {}


def run_layers(inp, x, layers):
    vp = pack_vecs(inp)
    vecs = vp.build()
    key = tuple(layers)
    if key not in _CACHE:
        _CACHE[key] = build_program(list(layers), vp.idx, vecs.shape[1])
    nc = _CACHE[key]
    consts = make_consts()
    wts = {}
    for l in layers:
        wts.update(layer_weights(inp, l))
    mem = np.asarray(inp["mem"], np.float32)
    pos = np.asarray(inp["positions"], np.int32)
    in_maps = []
    for b in range(8):
        m = {"x": np.ascontiguousarray(x[b]), "mem": np.ascontiguousarray(mem[b]),
             "pos": np.ascontiguousarray(pos[b:b + 1]), "consts": consts, "vecs": vecs}
        m.update(wts)
        in_maps.append(m)
    res = run_bass_kernel_spmd(nc, in_maps, core_ids=list(range(8)))
    return np.stack([np.asarray(r["y"], np.float32) for r in res.results], axis=0)


LAUNCH_PLAN = [[0, 1, 2, 3]]


def kernel(**inputs):
    x = np.asarray(inputs["x"], np.float32)
    for group in LAUNCH_PLAN:
        x = run_layers(inputs, x, group)
    return x
```

```python
import contextlib
import math
import numpy as np
import concourse.bass as bass
import concourse.mybir as mybir
from concourse.bass_utils import run_bass_kernel_spmd

F32 = mybir.dt.float32
BF16 = mybir.dt.bfloat16
I32 = mybir.dt.int32
AF = mybir.ActivationFunctionType
ALU = mybir.AluOpType

ENGS = ("pe", "act", "dve", "pool", "sp")
D = 1024
T = 2048
NG = 4
GS = 512
DEPTH = 4
KINDS = (0, 1, 2, 0)
RMS_EPS = 1e-6
LN_EPS = 1e-5
TWO_PI = 2.0 * math.pi


class Trk:
    __slots__ = ("w", "r")

    def __init__(self):
        self.w = None
        self.r = []


def trks(*shape):
    if len(shape) == 1:
        return [Trk() for _ in range(shape[0])]
    return [trks(*shape[1:]) for _ in range(shape[0])]


class Prog:
    def __init__(self, nc, n_dma_sems=32):
        self.nc = nc
        self.es = contextlib.ExitStack()
        self.streams = {e: [] for e in ENGS}
        self.count = {e: 0 for e in ENGS}
        self.pending = {e: False for e in ENGS}
        self.waited = {e: {} for e in ENGS}
        self.sems = {}
        for e in ("pe", "act", "dve", "pool"):
            self.sems[e] = self.es.enter_context(nc.semaphore("s_" + e))
        self.dma_sems = []
        for i in range(n_dma_sems):
            k = "d%d" % i
            self.sems[k] = self.es.enter_context(nc.semaphore("s_" + k))
            self.dma_sems.append(k)
        self.dma_val = {k: 0 for k in self.dma_sems}
        self.dma_rr = 0
        self.ntens = 0
        self.NSW = 4
        self.sw_sems = [self.es.enter_context(nc.semaphore("s_sw%d" % i)) for i in range(self.NSW)]
        self.sems["rel"] = self.es.enter_context(nc.semaphore("s_rel"))
        self.sw_issued = 0
        self.sw_relayed = 0

    def sbuf(self, shape, dtype):
        self.ntens += 1
        return self.es.enter_context(self.nc.sbuf_tensor("t%d" % self.ntens, list(shape), dtype))

    def psum(self, shape, dtype):
        self.ntens += 1
        return self.es.enter_context(self.nc.psum_tensor("p%d" % self.ntens, list(shape), dtype))

    @staticmethod
    def _deps(reads, writes):
        deps = {}

        def add(d):
            if d is not None and deps.get(d[0], 0) < d[1]:
                deps[d[0]] = d[1]
        for t in reads:
            add(t.w)
        for t in writes:
            add(t.w)
            for d in t.r:
                add(d)
        return deps

    def _emit_waits(self, eng, deps):
        for k, v in deps.items():
            if k == eng:
                if eng == "pe" or v <= self.count[eng] - 10 or v > self.count[eng]:
                    continue
            if self.waited[eng].get(k, 0) >= v:
                continue
            self.waited[eng][k] = v
            sem = self.sems[k]
            self.streams[eng].append(lambda e, sem=sem, v=v: e.wait_ge(sem, v))

    def _mark(self, tok, reads, writes):
        for t in reads:
            t.r.append(tok)
            if len(t.r) > 64:
                m = {}
                for k, v in t.r:
                    if m.get(k, 0) < v:
                        m[k] = v
                t.r = list(m.items())
        for t in writes:
            t.w = tok
            t.r = []

    log = None

    def op(self, eng, fn, reads=(), writes=(), inc=True, cost=None):
        if self.log is not None:
            self.log.append((eng, cost))
        self._emit_waits(eng, self._deps(reads, writes))
        tok = (eng, self.count[eng] + 1)
        if inc:
            sem = self.sems[eng]
            self.streams[eng].append(lambda e, fn=fn, sem=sem: fn(e).then_inc(sem, 1))
            self.count[eng] += 1
            self.pending[eng] = False
        else:
            self.streams[eng].append(lambda e, fn=fn: fn(e))
            self.pending[eng] = True
        self._mark(tok, reads, writes)
        return tok

    def dma(self, q, out, in_, reads=(), writes=()):
        deps = self._deps(reads, writes)
        k = self.dma_sems[self.dma_rr % len(self.dma_sems)]
        self.dma_rr += 1
        if self.dma_val[k] > 0:
            deps[k] = max(deps.get(k, 0), self.dma_val[k])
        self._emit_waits(q, deps)
        self.dma_val[k] += 16
        tok = (k, self.dma_val[k])
        sem = self.sems[k]
        self.streams[q].append(lambda e, out=out, in_=in_, sem=sem: e.dma_start(out=out, in_=in_).then_inc(sem, 16))
        self._mark(tok, reads, writes)
        return tok

    def dma_sw(self, out, in_, reads=(), writes=()):
        i = self.sw_issued
        if i - self.sw_relayed >= self.NSW:
            self.relay_upto(i - self.NSW)
        self._emit_waits("pool", self._deps(reads, writes))
        sem = self.sw_sems[i % self.NSW]
        self.streams["pool"].append(lambda e, out=out, in_=in_, sem=sem: e.dma_start(out=out, in_=in_).then_inc(sem, 16))
        self.sw_issued += 1
        self._mark(("rel", i + 1), reads, writes)
        return i

    def relay_upto(self, i):
        rel = self.sems["rel"]
        while self.sw_relayed <= min(i, self.sw_issued - 1):
            sem = self.sw_sems[self.sw_relayed % self.NSW]

            def f(e, sem=sem, rel=rel):
                e.wait_ge(sem, 16)
                e.sem_inc(sem, -16)
                e.sem_inc(rel, 1)
            self.streams["pool"].append(f)
            self.sw_relayed += 1

    def wait_tok(self, eng, toks):
        deps = {}
        for k, v in toks:
            if deps.get(k, 0) < v:
                deps[k] = v
        self._emit_waits(eng, deps)

    def emit(self):
        nc = self.nc
        self.relay_upto(self.sw_issued - 1)
        for e in ENGS:
            assert not self.pending[e], e
        with nc.Block() as block:
            @block.tensor
            def _(e):
                for f in self.streams["pe"]:
                    f(e)

            @block.scalar
            def _(e):
                for f in self.streams["act"]:
                    f(e)

            @block.vector
            def _(e):
                for f in self.streams["dve"]:
                    f(e)

            @block.gpsimd
            def _(e):
                for f in self.streams["pool"]:
                    f(e)

            @block.sync
            def _(e):
                for f in self.streams["sp"]:
                    f(e)
        self.es.close()


C_ID = 0
C_ONES = 128
C_BONES = 256
C_RT = 384
C_MASKA = 512
C_SWAM = 1024
NCB = 1280
C_IDF = 1280
C_CHM = 1408
C_INVF = 1920
C_EPSR = 1921
C_EPSL = 1922
C_HALFPI = 1923
NCONST = 1928


def make_consts():
    c = np.zeros((128, NCONST), np.float32)
    c[:, C_ID:C_ID + 128] = np.eye(128, dtype=np.float32)
    c[:, C_IDF:C_IDF + 128] = np.eye(128, dtype=np.float32)
    c[:, C_ONES:C_ONES + 128] = 1.0
    c[0:64, C_BONES:C_BONES + 64] = 1.0
    c[64:128, C_BONES + 64:C_BONES + 128] = 1.0
    rt = np.zeros((128, 128), np.float32)
    for blk in (0, 64):
        for d in range(32):
            rt[blk + d + 32, blk + d] = -1.0
            rt[blk + d, blk + d + 32] = 1.0
    c[:, C_RT:C_RT + 128] = rt
    j = np.arange(128)[:, None]
    i = np.arange(128)[None, :]
    ma = ((j // 64 == i // 64) & (j <= i)).astype(np.float32)
    c[:, C_MASKA:C_MASKA + 512] = np.tile(ma, (1, 4))
    c[:, C_SWAM:C_SWAM + 128] = (j > i).astype(np.float32)
    c[:, C_SWAM + 128:C_SWAM + 256] = (j <= i).astype(np.float32)
    chm = np.ones((128, 512), np.float32)
    chm[:, ::64] = 0.0
    c[:, C_CHM:C_CHM + 512] = chm
    invf = (10000.0 ** (-np.arange(32, dtype=np.float32) / np.float32(32))).astype(np.float32)
    c[:, C_INVF] = invf[np.arange(128) % 32]
    c[:, C_EPSR] = RMS_EPS
    c[:, C_EPSL] = LN_EPS
    c[:, C_HALFPI] = math.pi / 2
    return c


class VecPack:
    def __init__(self):
        self.cols = []
        self.idx = {}

    def add(self, name, v128xn):
        self.idx[name] = sum(a.shape[1] for a in self.cols)
        self.cols.append(np.ascontiguousarray(v128xn, dtype=np.float32))

    def add_feat(self, name, vec):
        v = np.asarray(vec, np.float32).reshape(-1, 128).T
        self.add(name, v)

    def build(self):
        return np.ascontiguousarray(np.concatenate(self.cols, axis=1))


def pack_vecs(inp):
    vp = VecPack()
    for l in range(DEPTH):
        vp.add_feat("nmix%d" % l, inp["norm_mix_g"][l])
        vp.add_feat("nmlp%d" % l, inp["norm_mlp_g"][l])
        vp.add_feat("nmem%d" % l, inp["mem_norm_g"][l])
        vp.add_feat("xaq%d" % l, inp["xa_q_norm_g"][l])
        vp.add_feat("xak%d" % l, inp["xa_k_norm_g"][l])
        vp.add_feat("lbl%d" % l, inp["hgrn_lb_logits"][l])
    for i in range(2):
        vp.add_feat("aon%d" % i, inp["a_o_norm_g"][i])
    vp.add_feat("bq", np.tile(inp["b_q_norm_g"][0], 2))
    vp.add_feat("bk", np.tile(inp["b_k_norm_g"][0], 2))
    vp.add_feat("sink", np.repeat(inp["b_sinks"][0], 64))
    vp.add_feat("cb", inp["c_conv_b"][0])
    vp.add_feat("lng", inp["c_ln_g"][0])
    vp.add_feat("lnb", inp["c_ln_b"][0])
    cw = np.asarray(inp["c_conv_w"][0], np.float32)
    vp.add("cw", cw.reshape(31, 8, 128).transpose(2, 1, 0).reshape(128, 8 * 31))
    return vp


def layer_weights(inp, l):
    kind = KINDS[l]
    idx = KINDS[:l].count(kind)
    if kind == 0:
        w = np.asarray(inp["a_w_in"][idx])
        cols = []
        for h in range(8):
            for s in range(4):
                cols.append(np.arange(s * 1024 + h * 128, s * 1024 + (h + 1) * 128))
        cols.append(np.arange(4096, 4608))
        win = w[:, np.concatenate(cols)]
        wout = inp["a_w_out"][idx]
    elif kind == 1:
        win = np.asarray(inp["b_w_in"][idx])
        wout = inp["b_w_out"][idx]
    else:
        w = np.asarray(inp["c_w_in"][idx])
        cols = []
        for blk in range(4):
            cols.append(np.arange(blk * 256, (blk + 1) * 256))
            cols.append(np.arange(1024 + blk * 256, 1024 + (blk + 1) * 256))
        cols.append(np.arange(2048, 2560))
        win = w[:, np.concatenate(cols)]
        wout = inp["c_w_out"][idx]
    return {
        "win%d" % l: np.ascontiguousarray(win, dtype=np.float32),
        "wout%d" % l: np.ascontiguousarray(wout, dtype=np.float32),
        "wup%d" % l: np.ascontiguousarray(inp["mlp_w_up"][l], dtype=np.float32),
        "wdn%d" % l: np.ascontiguousarray(inp["mlp_w_down"][l], dtype=np.float32),
        "wkv%d" % l: np.ascontiguousarray(inp["xa_w_kv"][l], dtype=np.float32),
    }


WIN_COLS = {0: 4608, 1: 1792, 2: 2560}


def build_program(layers, vidx, nv):
    nc = bass.Bass("TRN2", target_bir_lowering=False)
    x_d = nc.dram_tensor("x", [T, D], F32, kind="ExternalInput").ap()
    mem_d = nc.dram_tensor("mem", [256, D], F32, kind="ExternalInput").ap()
    pos_d = nc.dram_tensor("pos", [1, T], I32, kind="ExternalInput").ap()
    cst_d = nc.dram_tensor("consts", [128, NCONST], F32, kind="ExternalInput").ap()
    vec_d = nc.dram_tensor("vecs", [128, nv], F32, kind="ExternalInput").ap()
    y_d = nc.dram_tensor("y", [T, D], F32, kind="ExternalOutput").ap()
    wd = {}
    for l in layers:
        k = KINDS[l]
        wd["win%d" % l] = nc.dram_tensor("win%d" % l, [D, WIN_COLS[k]], F32, kind="ExternalInput").ap()
        wd["wout%d" % l] = nc.dram_tensor("wout%d" % l, [1536, D], F32, kind="ExternalInput").ap()
        wd["wup%d" % l] = nc.dram_tensor("wup%d" % l, [D, 4096], F32, kind="ExternalInput").ap()
        wd["wdn%d" % l] = nc.dram_tensor("wdn%d" % l, [4096, D], F32, kind="ExternalInput").ap()
        wd["wkv%d" % l] = nc.dram_tensor("wkv%d" % l, [D, D], F32, kind="ExternalInput").ap()

    P = Prog(nc)
    xT = P.sbuf([128, 8, T], F32)
    xTt = trks(8, NG)
    hT = P.sbuf([128, 8, T], BF16)
    hTt = trks(NG)
    mixT = P.sbuf([128, 12, T], BF16)
    mixt = trks(12, 16)
    cst_ = P.sbuf([128, NCONST - NCB], F32)
    cstt = Trk()
    cstb = P.sbuf([128, NCB], BF16)
    cstbt = Trk()

    class _CstView:
        def __getitem__(self, key):
            p, sl = key
            return cst_[p, sl.start - NCB:sl.stop - NCB]
    cst = _CstView()
    vec = P.sbuf([128, nv], F32)
    vect = Trk()
    kxa = P.sbuf([128, 4, 256], BF16)
    kxat = Trk()
    vxa = P.sbuf([128, 2, 512], BF16)
    vxat = Trk()
    small = P.sbuf([128, 64], F32)
    smallt = Trk()
    NSLOT = 2
    STG = 1024
    stage = [P.sbuf([128, STG], F32) for _ in range(2)]
    staget = trks(2)
    wslots = [P.sbuf([128, 4096], BF16) for _ in range(NSLOT)]
    wslott = trks(NSLOT)
    SCRW = 6656
    scr = P.sbuf([128, SCRW], F32)
    pb = [P.psum([128, 512], F32) for _ in range(8)]
    pbt = trks(8)
    state = {"bank": 0, "scr_trks": []}

    def V(name, j=0):
        c = vidx[name] + j
        return vec[:, c:c + 1]

    def CC(col):
        return cst[:, col:col + 1]

    POOLS = {"A": [0, 1, 2, 3], "B": [4, 5, 6, 7], "A1": [0], "A1g": [1], "A2": [2, 3, 4], "B3": [5, 6, 7]}

    def nb(pool=None):
        if pool is None:
            b = state["bank"]
            state["bank"] = (b + 1) % 8
            return b
        lst = POOLS[pool]
        k_ = "bank" + pool
        i_ = state.get(k_, 0)
        state[k_] = (i_ + 1) % len(lst)
        return lst[i_]

    class Scr:
        def __init__(self):
            self.off = 0
            self.t = []
            inh = {}
            for t in state["scr_trks"]:
                for d in ([t.w] if t.w else []) + t.r:
                    if inh.get(d[0], 0) < d[1]:
                        inh[d[0]] = d[1]
            self.inh = list(inh.items())

        def get(self, free_shape, dtype):
            n = int(np.prod(free_shape))
            words = n if dtype in (F32, I32) else (n + 1) // 2
            a = scr[:, self.off:self.off + words]
            self.off += words
            assert self.off <= SCRW, ("scratch overflow", self.off)
            if dtype != F32:
                a = a.bitcast(dtype)
            if len(free_shape) == 2:
                a = a.rearrange("p (a b) -> p a b", a=free_shape[0])
            elif len(free_shape) == 3:
                a = a.rearrange("p (a b c) -> p a b c", a=free_shape[0], b=free_shape[1])
            t = Trk()
            t.r = list(self.inh)
            self.t.append(t)
            return a, t

        def release(self, mark=0):
            inh = dict(self.inh)
            for t in self.t:
                for d in ([t.w] if t.w else []) + t.r:
                    if inh.get(d[0], 0) < d[1]:
                        inh[d[0]] = d[1]
            self.inh = list(inh.items())
            self.off = mark

        def close(self):
            state["scr_trks"] = self.t

    def MM(out, lhsT, rhs, start, stop, reads, writes, inc=None):
        if inc is None:
            inc = stop
        P.op("pe", lambda e: e.matmul(out, lhsT, rhs, start=start, stop=stop), reads, writes, inc,
             cost=0.06 + 0.00045 * int(np.prod(rhs.shape[1:])))

    def TRN(out, in_, ident, reads, writes, inc=True):
        P.op("pe", lambda e: e.transpose(out, in_, ident), reads, writes, inc)

    def ACT(out, in_, func, reads, writes, bias=None, scale=None, accum=None):
        kw = {}
        if bias is not None:
            kw["bias"] = bias
        if scale is not None:
            kw["scale"] = scale
        if accum is not None:
            kw["accum_out"] = accum
        P.op("act", lambda e: e.activation(out, in_, func, **kw), reads, writes,
             cost=0.25 + 0.0008 * int(np.prod(out.shape[1:])))

    def TT(eng, out, a, b, op, reads, writes):
        P.op(eng, lambda e: e.tensor_tensor(out, a, b, op), reads, writes,
             cost=(0.1 + 0.0012 * int(np.prod(out.shape[1:]))) * (1.6 if eng == "pool" else 1.0))

    def TS(eng, out, a, s1, s2, op0, op1, reads, writes):
        if s2 is None:
            P.op(eng, lambda e: e.tensor_scalar(out, a, s1, None, op0), reads, writes)
        else:
            P.op(eng, lambda e: e.tensor_scalar(out, a, s1, s2, op0, op1), reads, writes)

    def STT(out, a, s, b, op0, op1, reads, writes):
        P.op("dve", lambda e: e.scalar_tensor_tensor(out, a, s, b, op0, op1), reads, writes,
             cost=0.1 + 0.0012 * int(np.prod(out.shape[1:])))

    def CP(eng, out, in_, reads, writes):
        if eng == "act":
            ACT(out, in_, AF.Copy, reads, writes)
        else:
            P.op(eng, lambda e: e.tensor_copy(out, in_), reads, writes)

    def RECIP(out, in_, reads, writes):
        P.op("dve", lambda e: e.reciprocal(out, in_), reads, writes)

    idb = cstb[:, C_ID:C_ID + 128]
    onesb = cstb[:, C_ONES:C_ONES + 128]
    bonesb = cstb[:, C_BONES:C_BONES + 128]
    rtb = cstb[:, C_RT:C_RT + 128]
    idf = cst[:, C_IDF:C_IDF + 128]

    def gs(g):
        return slice(g * GS, (g + 1) * GS)

    def rstd_from(bank, out, outt, inv_n, epscol, n=GS, extra_reads=()):
        ACT(out, pb[bank][:, :n], AF.Ln, [pbt[bank], cstt] + list(extra_reads), [outt], bias=CC(epscol), scale=inv_n)
        ACT(out, out, AF.Exp, [outt], [outt], scale=-0.5)

    COST = {"pe": 0.12, "act": 0.65, "dve": 0.6, "pool": 1.0, "sp": 0.0}

    def pipeline_gen(n, *gens):
        gens = [g_ if isinstance(g_, tuple) else (g_, i_) for i_, g_ in enumerate(gens)]
        maxlag = max(lag for _, lag in gens)
        for k in range(n + maxlag):
            chains = []
            for gfn, lag in gens:
                if 0 <= k - lag < n:
                    chains.append([gfn(k - lag), 0.0])
            teng = {e: 0.0 for e in ENGS}
            while chains:
                ch = min(chains, key=lambda c_: c_[1])
                P.log = []
                try:
                    next(ch[0])
                except StopIteration:
                    chains.remove(ch)
                for eng, cst_ in P.log:
                    start = max(ch[1], teng[eng])
                    teng[eng] = start + (COST[eng] if cst_ is None else cst_)
                    ch[1] = teng[eng] + 0.15
            P.log = None

    def pipeline(n, stage1, stage2):
        for k in range(n + 1):
            if k < n:
                stage1(k)
            if k >= 1:
                stage2(k - 1)

    wq = []

    def wspec(l):
        k = KINDS[l]
        s = []
        wkv = wd["wkv%d" % l].rearrange("(k p) n -> p k n", p=128)
        win = wd["win%d" % l].rearrange("(k p) n -> p k n", p=128)
        wout = wd["wout%d" % l].rearrange("(k p) n -> p k n", p=128)
        wup = wd["wup%d" % l].rearrange("(k p) n -> p k n", p=128)
        wdn = wd["wdn%d" % l].rearrange("(k p) n -> p k n", p=128)
        pre = [(wkv[:, :, 0:512], 8, 512), (wkv[:, :, 512:1024], 8, 512)]
        ncols = WIN_COLS[k]
        c0 = 0
        if k == 1:
            widths = [512, 512, 256, 512]
        else:
            widths = [512] * (ncols // 512)
        for w_ in widths:
            s.append((win[:, :, c0:c0 + w_], 8, w_))
            c0 += w_
        for ob in range(4):
            s.append((wout[:, :, ob * 256:(ob + 1) * 256], 12, 256))
        m = []
        for hb in range(8):
            m.append((wup[:, :, hb * 512:(hb + 1) * 512], 8, 512))
            m.append((wdn[:, hb * 4:(hb + 1) * 4, :], 4, 1024))
        return pre, s, m

    specs = [wspec(l) for l in layers]
    for i_, (pre_, s_, m_) in enumerate(specs):
        if i_ == 0:
            wq.extend(pre_)
        wq.extend(s_)
        wq.extend(m_[0:4])
        if i_ + 1 < len(specs):
            wq.append(specs[i_ + 1][0][0])
        wq.extend(m_[4:8])
        if i_ + 1 < len(specs):
            wq.append(specs[i_ + 1][0][1])
        wq.extend(m_[8:])
    wstate = {"next_load": 0, "next_use": 0, "piece": 0}

    def w_prefetch(upto):
        while wstate["next_load"] < min(upto, len(wq)):
            i = wstate["next_load"]
            view, k, n = wq[i]
            slot = i % NSLOT
            dst = wslots[slot][:, 0:k * n].rearrange("p (k n) -> p k n", k=k)
            kk = max(1, STG // n)
            for k0 in range(0, k, kk):
                k1 = min(k, k0 + kk)
                j = wstate["piece"] % 2
                wstate["piece"] += 1
                stv = stage[j][:, 0:(k1 - k0) * n].rearrange("p (k n) -> p k n", k=k1 - k0)
                P.dma("sp", stv, view[:, k0:k1, :], reads=[], writes=[staget[j]])
                P.op("pool", lambda e, o=dst[:, k0:k1, :], i_=stv: e.tensor_copy(o, i_), [staget[j]], [wslott[slot]])
            wstate["next_load"] += 1

    def w_next(k, n, prefetch=True):
        i = wstate["next_use"]
        assert wq[i][1] == k and wq[i][2] == n, (i, wq[i][1:], k, n)
        w_prefetch(i + NSLOT if prefetch else i + 1)
        wstate["next_use"] += 1
        slot = i % NSLOT
        return wslots[slot][:, 0:k * n].rearrange("p (k n) -> p k n", k=k), wslott[slot]

    P.dma("sp", cst_[:], cst_d[:, NCB:NCONST], writes=[cstt])
    P.dma("sp", vec[:], vec_d, writes=[vect])
    sc = Scr()
    ctmp, ctmpt = sc.get([NCB], F32)
    P.dma("sp", ctmp, cst_d[:, 0:NCB], writes=[ctmpt])
    CP("dve", cstb[:], ctmp, [ctmpt], [cstbt])
    w_prefetch(NSLOT)
    xin, xint = [], []
    for i in range(2):
        a, t = sc.get([D], F32)
        xin.append(a)
        xint.append(t)
    for i in range(16):
        s_ = i % 2
        P.dma("sp", xin[s_], x_d[i * 128:(i + 1) * 128, :], writes=[xint[s_]])
        for half in range(2):
            b = nb()
            for c4 in range(4):
                c = half * 4 + c4
                TRN(pb[b][:, c4 * 128:(c4 + 1) * 128], xin[s_][:, c * 128:(c + 1) * 128], idf,
                    [xint[s_], cstt], [pbt[b]], inc=(c4 == 3))
            g = i // 4
            wr = [xTt[half * 4 + c4][g] for c4 in range(4)]
            CP("dve" if half == 0 else "act", xT[:, half * 4:half * 4 + 4, i * 128:(i + 1) * 128],
               pb[b][:].rearrange("p (c t) -> p c t", c=4), [pbt[b]], wr)
    sc.close()

    def prenorm(gname):
        sc = Scr()
        sqs = [sc.get([8, GS], BF16) for _ in range(2)]
        rs = [sc.get([GS], F32) for _ in range(2)]
        banks = {}

        def s1(g):
            sq, sqt = sqs[g % 2]
            for c in range(8):
                ACT(sq[:, c, :], xT[:, c, gs(g)], AF.Square, [xTt[c][g]], [sqt])
            b = nb()
            banks[g] = b
            for c in range(8):
                MM(pb[b][:], onesb, sq[:, c, :], c == 0, c == 7, [sqt, cstbt], [pbt[b]])

        def s2(g):
            r, rt = rs[g % 2]
            rstd_from(banks[g], r, rt, 1.0 / D, C_EPSR)
            for c in range(8):
                STT(hT[:, c, gs(g)], xT[:, c, gs(g)], V(gname, c), r, ALU.mult, ALU.mult,
                    [xTt[c][g], rt, vect], [hTt[g]])
        pipeline(NG, s1, s2)
        sc.close()

    def proj_fm(wt, wtt, c0, g, pool=None):
        b = nb(pool)
        for kc in range(8):
            MM(pb[b][:], wt[:, kc, c0:c0 + 128], hT[:, kc, gs(g)], kc == 0, kc == 7, [wtt, hTt[g]], [pbt[b]])
        return b

    def proj_gen(wt, wtt, c0, g, pool):
        b = nb(pool)
        for kc in range(8):
            MM(pb[b][:], wt[:, kc, c0:c0 + 128], hT[:, kc, gs(g)], kc == 0, kc == 7, [wtt, hTt[g]], [pbt[b]])
            if kc == 3:
                yield
        return b

    def mt_grp(c, g):
        return mixt[c][4 * g:4 * g + 4]

    def xa_prep_p1(l, sc):
        mm_, mmt = sc.get([2, D], F32)
        junk, junkt = sc.get([D], BF16)
        memn, memnt = sc.get([8, 256], BF16)
        ksq, ksqt = sc.get([4, 256], BF16)
        krs, krst = sc.get([4, 256], F32)
        ssq = small[:, 0:2]
        for mt in range(2):
            P.dma("sp", mm_[:, mt, :], mem_d[mt * 128:(mt + 1) * 128, :], writes=[mmt])
        for mt in range(2):
            ACT(junk, mm_[:, mt, :], AF.Square, [mmt], [junkt, smallt], accum=ssq[:, mt:mt + 1])
        ACT(ssq, ssq, AF.Ln, [smallt, cstt], [smallt], bias=CC(C_EPSR), scale=1.0 / D)
        ACT(ssq, ssq, AF.Exp, [smallt], [smallt], scale=-0.5)
        for mt in range(2):
            TS("dve", mm_[:, mt, :], mm_[:, mt, :], ssq[:, mt:mt + 1], None, ALU.mult, None, [mmt, smallt], [mmt])
        gkq = small[:, 2:3]
        TS("dve", gkq, V("xak%d" % l), V("xaq%d" % l), 128.0 ** -0.5, ALU.mult, ALU.mult, [vect], [smallt])
        return (mm_, mmt, memn, memnt, ksq, ksqt, krs, krst, gkq)

    def xa_prep_p2a(l, bufs):
        mm_, mmt, memn, memnt, ksq, ksqt, krs, krst, gkq = bufs
        for c in range(8):
            b = nb()
            for mt in range(2):
                TRN(pb[b][:, mt * 128:(mt + 1) * 128], mm_[:, mt, c * 128:(c + 1) * 128], idf, [mmt, cstt], [pbt[b]],
                    inc=(mt == 1))
            yield
            TS("dve", memn[:, c, :], pb[b][:, 0:256], V("nmem%d" % l, c), None, ALU.mult, None, [pbt[b], vect], [memnt])
            yield

    def xa_prep_p2(l, bufs):
        mm_, mmt, memn, memnt, ksq, ksqt, krs, krst, gkq = bufs
        wk, wkt = w_next(8, 512)
        kb = []
        for h in range(4):
            if h % 2 == 0:
                b = nb()
                kb.append(b)
            for kc in range(8):
                MM(pb[b][:, (h % 2) * 256:(h % 2 + 1) * 256], wk[:, kc, h * 128:(h + 1) * 128], memn[:, kc, :],
                   kc == 0, kc == 7, [wkt, memnt], [pbt[b]])
        for hp in range(2):
            ACT(ksq[:, 2 * hp:2 * hp + 2, :], pb[kb[hp]][:].rearrange("p (a b) -> p a b", a=2), AF.Square,
                [pbt[kb[hp]]], [ksqt])
        for hp in range(2):
            b = nb()
            MM(pb[b][:], onesb, ksq[:, 2 * hp:2 * hp + 2, :], True, True, [ksqt, cstbt], [pbt[b]])
            kr = krs[:, 2 * hp:2 * hp + 2, :]
            ACT(kr, pb[b][:].rearrange("p (a b) -> p a b", a=2), AF.Ln, [pbt[b], cstt], [krst],
                bias=CC(C_EPSR), scale=1.0 / 128)
            ACT(kr, kr, AF.Exp, [krst], [krst], scale=-0.5)
            STT(kxa[:, 2 * hp:2 * hp + 2, :], pb[kb[hp]][:].rearrange("p (a b) -> p a b", a=2), gkq, kr,
                ALU.mult, ALU.mult, [pbt[kb[hp]], krst, smallt], [kxat])

    def xa_prep_p2v(l, bufs):
        mm_, mmt, memn, memnt, ksq, ksqt, krs, krst, gkq = bufs
        wv, wvt = w_next(8, 512)
        for mt in range(2):
            b = nb()
            for kc in range(8):
                MM(pb[b][:], memn[:, kc, mt * 128:(mt + 1) * 128], wv[:, kc, :], kc == 0, kc == 7,
                   [wvt, memnt], [pbt[b]])
            CP("act", vxa[:, mt, :], pb[b][:], [pbt[b]], [vxat])

    def xa_attend(sc, wt, wtt):
        sqs = [sc.get([GS], BF16) for _ in range(2)]
        rss = [sc.get([GS], F32) for _ in range(2)]
        ee = [sc.get([2, 256], BF16) for _ in range(2)]
        rds = [sc.get([256], F32) for _ in range(2)]
        st = {}

        def q1(k):
            h, g = divmod(k, NG)
            sq, sqt = sqs[k % 2]
            bq = proj_fm(wt, wtt, h * 128, g)
            ACT(sq, pb[bq][:], AF.Square, [pbt[bq]], [sqt])
            bs = nb()
            MM(pb[bs][:], onesb, sq, True, True, [sqt, cstbt], [pbt[bs]])
            st[k] = (bq, bs)

        def q2(k):
            h, g = divmod(k, NG)
            bq, bs = st[k]
            rs, rst = rss[k % 2]
            rstd_from(bs, rs, rst, 1.0 / 128, C_EPSR)
            TT("dve", mixT[:, 8 + h, gs(g)], pb[bq][:], rs, ALU.mult, [pbt[bq], rst], mt_grp(8 + h, g))
        pipeline(16, q1, q2)

        def a1(k):
            h, r_ = divmod(k, 8)
            cols = slice(r_ * 256, (r_ + 1) * 256)
            qt = mixt[8 + h][2 * r_:2 * r_ + 2]
            e, et = ee[k % 2]
            b = nb()
            for mt in range(2):
                MM(pb[b][:, mt * 256:(mt + 1) * 256], kxa[:, h, mt * 128:(mt + 1) * 128], mixT[:, 8 + h, cols], True, True,
                   [kxat] + qt, [pbt[b]], inc=(mt == 1))
            ACT(e, pb[b][:].rearrange("p (a b) -> p a b", a=2), AF.Exp, [pbt[b]], [et])

        def a2(k):
            h, r_ = divmod(k, 8)
            cols = slice(r_ * 256, (r_ + 1) * 256)
            qt = mixt[8 + h][2 * r_:2 * r_ + 2]
            e, et = ee[k % 2]
            rd, rdt = rds[k % 2]
            bn = nb()
            for mt in range(2):
                MM(pb[bn][:, 0:256], vxa[:, mt, h * 128:(h + 1) * 128], e[:, mt, :], mt == 0, mt == 1, [vxat, et], [pbt[bn]],
                   inc=False)
            for mt in range(2):
                MM(pb[bn][:, 256:512], onesb, e[:, mt, :], mt == 0, mt == 1, [cstbt, et], [pbt[bn]], inc=(mt == 1))
            ACT(rd, pb[bn][:, 256:512], AF.Ln, [pbt[bn]], [rdt])
            ACT(rd, rd, AF.Exp, [rdt], [rdt], scale=-1.0)
            TT("dve", mixT[:, 8 + h, cols], pb[bn][:, 0:256], rd, ALU.mult, [pbt[bn], rdt], qt)
        pipeline(32, a1, a2)

    def out_proj(l):
        for ob in range(4):
            wt, wtt = w_next(12, 256)
            for oo in range(2):
                o = ob * 2 + oo
                for g in range(NG):
                    b = nb()
                    for kc in range(12):
                        MM(pb[b][:], wt[:, kc, oo * 128:(oo + 1) * 128], mixT[:, kc, gs(g)], kc == 0, kc == 11,
                           [wtt] + mt_grp(kc, g), [pbt[b]])
                    TT("dve", xT[:, o, gs(g)], pb[b][:], xT[:, o, gs(g)], ALU.add, [pbt[b], xTt[o][g]], [xTt[o][g]])

    def mlp(l, next_l=None):
        prenorm("nmlp%d" % l)
        sc = Scr()
        rl = [sc.get([GS], F32) for _ in range(3)]
        xbufs = xa_prep_p1(next_l, sc) if next_l is not None else None
        xgen = xa_prep_p2a(next_l, xbufs) if next_l is not None else iter(())

        def xstep():
            try:
                next(xgen)
            except StopIteration:
                pass
        u = 0
        for hb in range(8):
            if hb == 2 and xbufs is not None:
                for _ in xgen:
                    pass
                xa_prep_p2(next_l, xbufs)
            if hb == 4 and xbufs is not None:
                xa_prep_p2v(next_l, xbufs)
            wu, wut = w_next(8, 512)
            ab = (hb % 2) * 4
            for j in range(4):
                for g in range(NG):
                    b = proj_fm(wu, wut, j * 128, g)
                    r, rt = rl[u % 3]
                    u += 1
                    ACT(r, pb[b][:], AF.Relu, [pbt[b]], [rt])
                    ACT(mixT[:, ab + j, gs(g)], r, AF.Square, [rt], mt_grp(ab + j, g))
                    if hb >= 1:
                        xstep()
            wdt, wdtt = w_next(4, 1024)
            for o in range(8):
                for g in range(NG):
                    b = nb()
                    for j in range(4):
                        MM(pb[b][:], wdt[:, j, o * 128:(o + 1) * 128], mixT[:, ab + j, gs(g)], j == 0, j == 3,
                           [wdtt] + mt_grp(ab + j, g), [pbt[b]])
                    TT("dve", xT[:, o, gs(g)], pb[b][:], xT[:, o, gs(g)], ALU.add, [pbt[b], xTt[o][g]], [xTt[o][g]])
        sc.close()

    def lb_prep(l):
        e_ = small[:, 24:56].rearrange("p (l c) -> p l c", l=4)
        for ll in range(4):
            ACT(e_[:, ll, :], vec[:, vidx["lbl%d" % ll]:vidx["lbl%d" % ll] + 8], AF.Exp, [vect], [smallt])
        tot = small[:, 56:64]
        TT("dve", tot, e_[:, 0, :], e_[:, 1, :], ALU.add, [smallt], [smallt])
        TT("dve", tot, tot, e_[:, 2, :], ALU.add, [smallt], [smallt])
        TT("dve", tot, tot, e_[:, 3, :], ALU.add, [smallt], [smallt])
        RECIP(tot, tot, [smallt], [smallt])
        lb = small[:, 8:16]
        P.op("dve", lambda e: e.memset(lb, 0.0), [], [smallt])
        for ll in range(1, l + 1):
            TT("dve", lb, lb, e_[:, ll, :], ALU.add, [smallt], [smallt])
        TT("dve", lb, lb, tot, ALU.mult, [smallt], [smallt])
        oml = small[:, 16:24]
        TS("dve", oml, lb, -1.0, 1.0, ALU.mult, ALU.add, [smallt], [smallt])

    def mixer_hgrn(l):
        idx = KINDS[:l].count(0)
        lb_prep(l)
        sc = Scr()
        Fs = [[sc.get([GS], F32) for _ in range(4)] for _ in range(2)]
        qes = [sc.get([GS], BF16) for _ in range(3)]
        kebs = [sc.get([GS], BF16) for _ in range(3)]
        sgts = [sc.get([GS], BF16) for _ in range(3)]
        xflat = mixT[:, 8:12, :].rearrange("p c t -> p (c t)").bitcast(F32)
        xinh = []
        for c_ in range(8, 12):
            for m_ in mixt[c_]:
                xinh.extend(([m_.w] if m_.w else []) + m_.r)
        xst = {"off": 0, "t": []}

        def xget(free_shape, dtype):
            n_ = int(np.prod(free_shape))
            words = n_ if dtype == F32 else (n_ + 1) // 2
            a_ = xflat[:, xst["off"]:xst["off"] + words]
            xst["off"] += words
            assert xst["off"] <= 4096
            if dtype != F32:
                a_ = a_.bitcast(dtype)
            if len(free_shape) == 2:
                a_ = a_.rearrange("p (a b) -> p a b", a=free_shape[0])
            t_ = Trk()
            t_.r = list(xinh)
            xst["t"].append(t_)
            return a_, t_
        Sall, _ = xget([9, 128], F32)
        Sallt = [Trk() for _ in range(9)]
        for t_ in Sallt:
            t_.r = list(xinh)
            xst["t"].append(t_)
        k2ts = [xget([4, 128], BF16) for _ in range(3)]
        vtks = [xget([4, 128], BF16) for _ in range(3)]
        ees = [xget([16], F32) for _ in range(3)]
        R1, R1t = xget([GS], F32)
        am, amt = xget([GS], BF16)
        sta, stat = xget([8, 128], BF16)
        stats = [stat] + [Trk() for _ in range(7)]
        for t_ in stats[1:]:
            t_.r = list(xinh)
            xst["t"].append(t_)
        chm = cst[:, C_CHM:C_CHM + GS]
        maskA = cstb[:, C_MASKA:C_MASKA + GS]
        LN_MIN = math.log(1e-20)
        wts = {}
        banks = {}

        def sA1(k):
            h, g = divmod(k, NG)
            if g == 0:
                wts[h] = w_next(8, 512, prefetch=(h == 0))
            wt, wtt = wts[h]
            (F1, F1t), (F2, F2t), (F3, F3t), (F4, F4t) = Fs[k % 2]
            lbc = small[:, 8 + h:9 + h]
            bf = yield from proj_gen(wt, wtt, 128, g, "A1")
            yield
            ACT(F1, pb[bf][:], AF.Exp, [pbt[bf]], [F1t], scale=-1.0)
            ACT(F2, F1, AF.Ln, [F1t], [F2t], bias=1.0)
            ACT(F3, F1, AF.Ln, [F1t, smallt], [F3t], bias=1.0, scale=lbc)
            yield
            STT(F3, F2, -1.0, F3, ALU.mult, ALU.add, [F2t, F3t], [F3t])
            TS("dve", F3, F3, LN_MIN, None, ALU.max, None, [F3t], [F3t])
            STT(F1, pb[bf][:], -1.0, F2, ALU.mult, ALU.subtract, [pbt[bf], F2t], [F1t])
            yield
            ACT(F1, F1, AF.Exp, [F1t], [F1t])

        def sA1g(k):
            h, g = divmod(k, NG)
            wt, wtt = wts[h]
            (F1, F1t), (F2, F2t), (F3, F3t), (F4, F4t) = Fs[k % 2]
            sgt_, sgtt = sgts[k % 3]
            bg = yield from proj_gen(wt, wtt, 384, g, "A1g")
            yield
            ACT(F4, pb[bg][:], AF.Exp, [pbt[bg]], [F4t], scale=-1.0)
            yield
            ACT(F4, F4, AF.Ln, [F4t], [F4t], bias=1.0)
            yield
            ACT(F4, F4, AF.Exp, [F4t], [F4t], scale=-1.0)
            yield
            STT(sgt_, pb[bg][:], V("aon%d" % idx, h), F4, ALU.mult, ALU.mult, [pbt[bg], F4t, vect], [sgtt])

        def sA2(k):
            h, g = divmod(k, NG)
            wt, wtt = wts[h]
            (F1, F1t), (F2, F2t), (F3, F3t), (F4, F4t) = Fs[k % 2]
            qe, qet = qes[k % 3]
            keb, kebt = kebs[k % 3]
            k2t, k2tt = k2ts[k % 3]
            vtk, vtkt = vtks[k % 3]
            ee, eet = ees[k % 3]
            omc = small[:, 16 + h:17 + h]
            P.op("dve", lambda e: e.tensor_tensor_scan(F2, chm, F3, 0.0, ALU.mult, ALU.add), [cstt, F3t], [F2t])
            b3 = F2.rearrange("p (c t) -> p c t", c=8)
            bp3 = F3.rearrange("p (c t) -> p c t", c=8)
            TT("dve", bp3, b3, b3[:, :, 31:32].to_broadcast([128, 8, 64]), ALU.subtract, [F2t], [F3t])
            yield
            bq = yield from proj_gen(wt, wtt, 0, g, "A2")
            yield
            ACT(F4, F3, AF.Exp, [F3t], [F4t])
            ACT(ee[:, 0:8], b3[:, :, 63], AF.Exp, [F2t], [eet])
            ACT(ee[:, 8:16], b3[:, :, 31], AF.Exp, [F2t], [eet])
            ACT(F2, F3, AF.Exp, [F3t], [F2t], scale=-1.0)
            yield
            bv = nb("A2")
            for i in range(4):
                for kc in range(8):
                    MM(pb[bv][:, i * 128:(i + 1) * 128], hT[:, kc, g * GS + i * 128:g * GS + (i + 1) * 128],
                       wt[:, kc, 256:384], kc == 0, kc == 7, [wtt, hTt[g]], [pbt[bv]], inc=(kc == 7 and i == 3))
                yield
            TT("dve", qe, pb[bq][:], F4, ALU.mult, [pbt[bq], F4t], [qet])
            STT(keb, F1, omc, F2, ALU.mult, ALU.mult, [F1t, F2t, smallt], [kebt])
            yield
            eb3 = F4.rearrange("p (c t) -> p c t", c=8)
            TT("pool", F3.rearrange("p (c t) -> p c t", c=8), keb.rearrange("p (c t) -> p c t", c=8),
               eb3[:, :, 63:64].to_broadcast([128, 8, 64]), ALU.mult, [kebt, F4t], [F3t])
            CP("act", vtk, pb[bv][:].rearrange("p (a b) -> p a b", a=4), [pbt[bv]], [vtkt])
            yield
            bt = nb("A2")
            for i in range(4):
                TRN(pb[bt][:, i * 128:(i + 1) * 128], F3[:, i * 128:(i + 1) * 128], idf, [F3t, cstt], [pbt[bt]],
                    inc=(i == 3))
            yield
            CP("act", k2t, pb[bt][:].rearrange("p (a b) -> p a b", a=4), [pbt[bt]], [k2tt])
            if g == NG - 1:
                w_prefetch(wstate["next_use"] + 1)

        def sB(k):
            h, g = divmod(k, NG)
            qe, qet = qes[k % 3]
            keb, kebt = kebs[k % 3]
            sgt_, sgtt = sgts[k % 3]
            k2t, k2tt = k2ts[k % 3]
            vtk, vtkt = vtks[k % 3]
            ee, eet = ees[k % 3]
            if g == 0:
                P.op("dve", lambda e: e.memset(Sall[:, 0, :], 0.0), [], [Sallt[0]])
            else:
                CP("dve", Sall[:, 0, :], Sall[:, 8, :], [Sallt[8]], [Sallt[0]])
            bk = [nb("B3"), nb("B3")]
            for par in range(2):
                for i in range(4):
                    MM(pb[bk[par]][:, i * 128:(i + 1) * 128], k2t[par * 64:(par + 1) * 64, i, :],
                       vtk[par * 64:(par + 1) * 64, i, :], True, True, [k2tt, vtkt], [pbt[bk[par]]], inc=(i == 3))
            yield
            ba = nb("B3")
            for i in range(4):
                MM(pb[ba][:, i * 128:(i + 1) * 128], keb[:, i * 128:(i + 1) * 128], qe[:, i * 128:(i + 1) * 128],
                   True, True, [kebt, qet], [pbt[ba]], inc=(i == 3))
            yield
            for cc in range(8):
                ACT(sta[:, cc, :], Sall[:, cc, :], AF.Copy, [Sallt[cc], eet], [stats[cc]], scale=ee[:, 8 + cc:9 + cc])
                STT(Sall[:, cc + 1, :], Sall[:, cc, :], ee[:, cc:cc + 1],
                    pb[bk[cc % 2]][:, (cc // 2) * 128:(cc // 2 + 1) * 128],
                    ALU.mult, ALU.add, [Sallt[cc], eet, pbt[bk[cc % 2]]], [Sallt[cc + 1]])
                if cc == 1:
                    TT("dve", am, pb[ba][:], maskA, ALU.mult, [pbt[ba], cstbt], [amt])
                yield
            bo = nb("B3")
            for i in range(4):
                MM(pb[bo][:, i * 128:(i + 1) * 128], vtk[:, i, :], am[:, i * 128:(i + 1) * 128], True, False,
                   [vtkt, amt], [pbt[bo]], inc=False)
                for hh in range(2):
                    cc = 2 * i + hh
                    MM(pb[bo][:, cc * 64:(cc + 1) * 64], sta[:, cc, :], qe[:, cc * 64:(cc + 1) * 64], False, hh == 1,
                       [stats[cc], qet], [pbt[bo]], inc=(hh == 1 and i == 3))
            yield
            ACT(am, pb[bo][:], AF.Square, [pbt[bo]], [amt])
            yield
            bs = nb("B3")
            MM(pb[bs][:], onesb, am, True, True, [amt, cstbt], [pbt[bs]])
            yield
            rstd_from(bs, R1, R1t, 1.0 / 128, C_EPSR)
            yield
            TT("dve", R1, pb[bo][:], R1, ALU.mult, [pbt[bo], R1t], [R1t])
            TT("dve", mixT[:, h, gs(g)], R1, sgt_, ALU.mult, [R1t, sgtt], mt_grp(h, g))

        pipeline_gen(32, (sA1, 0), (sA1g, 0), (sA2, 1), (sB, 2))
        for t_ in xst["t"]:
            for c_ in range(8, 12):
                for m_ in mixt[c_]:
                    m_.r.extend(([t_.w] if t_.w else []) + t_.r)
        wt, wtt = w_next(8, 512)
        sc.release(0)
        xa_attend(sc, wt, wtt)
        sc.close()

    def mixer_swa(l):
        sc = Scr()
        kT = [(mixT[:, 8, :], Trk()), (mixT[:, 9, :], Trk())]
        vtk, vtkt = mixT[:, 10, :].rearrange("p (a b) -> p a b", a=16), Trk()
        cosT, cost = mixT[:, 11, :], Trk()
        for t_ in (kT[0][1], kT[1][1], vtkt, cost):
            for c_ in range(8, 12):
                for m_ in mixt[c_]:
                    t_.r.extend(([m_.w] if m_.w else []) + m_.r)
        sinT, sint = sc.get([T], BF16)
        esk = small[:, 8:16]
        ACT(esk, vec[:, vidx["sink"]:vidx["sink"] + 8], AF.Exp, [vect], [smallt])
        sc_mark = sc.off
        pi_, pit = sc.get([GS], I32)
        a1, a1t = sc.get([GS], F32)
        a2, a2t = sc.get([GS], F32)
        a3, a3t = sc.get([GS], F32)
        MAGIC = 12582912.0
        C1 = 6.28125
        C2 = TWO_PI - 6.28125
        for g in range(NG):
            P.dma("sp", pi_, pos_d[0:1, gs(g)].partition_broadcast(128), writes=[pit])
            CP("dve", a1, pi_, [pit], [a1t])
            TS("dve", a1, a1, CC(C_INVF), None, ALU.mult, None, [a1t, cstt], [a1t])
            for which, dst, dstt in ((0, sinT, sint), (1, cosT, cost)):
                if which == 1:
                    TS("dve", a1, a1, CC(C_HALFPI), None, ALU.add, None, [a1t, cstt], [a1t])
                TS("dve", a2, a1, 1.0 / TWO_PI, MAGIC, ALU.mult, ALU.add, [a1t], [a2t])
                TS("dve", a2, a2, -MAGIC, None, ALU.add, None, [a2t], [a2t])
                STT(a3, a2, -C1, a1, ALU.mult, ALU.add, [a2t, a1t], [a3t])
                STT(a3, a2, -C2, a3, ALU.mult, ALU.add, [a2t, a3t], [a3t])
                TS("dve", a3, a3, math.pi, -math.pi, ALU.min, ALU.max, [a3t], [a3t])
                ACT(dst[:, gs(g)], a3, AF.Sin, [a3t], [dstt])
        sc.release(sc_mark)
        sqs = [sc.get([GS], BF16) for _ in range(2)]
        qgs = [sc.get([GS], BF16) for _ in range(2)]
        rss = [sc.get([GS], F32) for _ in range(2)]
        t1s = [sc.get([GS], F32) for _ in range(2)]
        t2s = [sc.get([GS], F32) for _ in range(2)]
        units = []
        for tix in range(2):
            for cj in range(4):
                for g in range(NG):
                    units.append(("q", tix, cj, g))
        for gk in range(2):
            for g in range(NG):
                units.append(("k", gk, 0, g))
        wcache = {}
        st = {}

        def get_w(key, k_, n_):
            if key not in wcache:
                wcache[key] = w_next(k_, n_)
            return wcache[key]

        def r1(k):
            kind_, a_, cj, g = units[k]
            if kind_ == "q":
                wt, wtt = get_w(("q", a_), 8, 512)
                b = proj_fm(wt, wtt, cj * 128, g)
                gcol = V("bq")
            else:
                wt, wtt = get_w("kv", 8, 256)
                b = nb()
                for rep in range(2):
                    for kc in range(8):
                        MM(pb[b][rep * 64:(rep + 1) * 64, :], wt[:, kc, a_ * 64:(a_ + 1) * 64], hT[:, kc, gs(g)],
                           kc == 0, kc == 7, [wtt, hTt[g]], [pbt[b]], inc=(kc == 7 and rep == 1))
                gcol = V("bk")
            sq, sqt = sqs[k % 2]
            qg, qgt = qgs[k % 2]
            ACT(sq, pb[b][:], AF.Square, [pbt[b]], [sqt])
            ACT(qg, pb[b][:], AF.Copy, [pbt[b], vect], [qgt], scale=gcol)
            bm = nb()
            MM(pb[bm][:], bonesb, sq, True, True, [sqt, cstbt], [pbt[bm]])
            br = nb()
            MM(pb[br][:], rtb, qg, True, True, [qgt, cstbt], [pbt[br]])
            st[k] = (bm, br)

        def r2(k):
            kind_, a_, cj, g = units[k]
            bm, br = st[k]
            qg, qgt = qgs[k % 2]
            rs, rst = rss[k % 2]
            t1, t1t = t1s[k % 2]
            t2, t2t = t2s[k % 2]
            if kind_ == "q":
                c = a_ * 4 + cj
                dst, dstt_list = mixT[:, c, gs(g)], mt_grp(c, g)
            else:
                dst, dstt_list = kT[a_][0][:, gs(g)], [kT[a_][1]]
            rstd_from(bm, rs, rst, 1.0 / 64, C_EPSR)
            TT("pool", t1, qg, cosT[:, gs(g)], ALU.mult, [qgt, cost], [t1t])
            TT("dve", t2, pb[br][:], sinT[:, gs(g)], ALU.mult, [pbt[br], sint], [t2t])
            TT("dve", t1, t1, t2, ALU.add, [t1t, t2t], [t1t])
            TT("dve", dst, t1, rs, ALU.mult, [t1t, rst], dstt_list)
        pipeline(len(units), r1, r2)
        wt, wtt = get_w("kv", 8, 256)
        for i4 in range(4):
            b = nb()
            for ii in range(4):
                i = i4 * 4 + ii
                for kc in range(8):
                    MM(pb[b][:, ii * 128:(ii + 1) * 128], hT[:, kc, i * 128:(i + 1) * 128], wt[:, kc, 128:256],
                       kc == 0, kc == 7, [wtt, hTt[i4]], [pbt[b]], inc=(kc == 7 and ii == 3))
            CP("act", vtk[:, i4 * 4:i4 * 4 + 4, :], pb[b][:].rearrange("p (a b) -> p a b", a=4), [pbt[b]], [vtkt])
        sc.release(sc_mark)
        EE = []
        for _ in range(2):
            e_, t0_ = sc.get([2, 2, 128], BF16)
            t1_ = Trk()
            t1_.r = list(t0_.r)
            sc.t.append(t1_)
            EE.append((e_, (t0_, t1_)))
        dn_ = [sc.get([128], F32) for _ in range(2)]
        swam = cstb[:, C_SWAM:C_SWAM + 256].rearrange("p (a b) -> p a b", a=2)

        def c1(u):
            c, n = divmod(u, 16)
            gk = c // 4
            kTa, kTt = kT[gk]
            kbs = [1] if n == 0 else [0, 1]
            e, et = EE[u % 2]
            q_t = [mixt[c][n]]
            for par in range(2):
                b = nb("A")
                for kb_ in kbs:
                    nbk = n - 1 + kb_
                    MM(pb[b][:, kb_ * 128:(kb_ + 1) * 128], kTa[par * 64:(par + 1) * 64, nbk * 128:(nbk + 1) * 128],
                       mixT[par * 64:(par + 1) * 64, c, n * 128:(n + 1) * 128], True, True, [kTt] + q_t, [pbt[b]],
                       inc=(kb_ == 1))
                lo = kbs[0]
                ACT(e[:, par, lo:2, :], pb[b][:, lo * 128:256].rearrange("p (a b) -> p a b", b=128), AF.Exp,
                    [pbt[b]], [et[par]], scale=0.125)
                TT("pool" if par == 0 else "dve", e[:, par, lo:2, :], e[:, par, lo:2, :], swam[:, lo:2, :], ALU.mult,
                   [et[par], cstbt], [et[par]])
                yield

        def c2(u):
            c, n = divmod(u, 16)
            gk = c // 4
            kbs = [1] if n == 0 else [0, 1]
            e, et = EE[u % 2]
            dn, dnt = dn_[u % 2]
            bn = nb("B")
            for par in range(2):
                for which in range(2):
                    for kb_ in kbs:
                        nbk = n - 1 + kb_
                        lhs = vtk[:, nbk, gk * 64:(gk + 1) * 64] if which == 0 else onesb[:, 0:64]
                        MM(pb[bn][par * 64:(par + 1) * 64, which * 128:(which + 1) * 128], lhs, e[:, par, kb_, :],
                           kb_ == kbs[0], kb_ == 1, [vtkt, cstbt, et[par]], [pbt[bn]],
                           inc=(kb_ == 1 and which == 1 and par == 1))
            yield
            ACT(dn, pb[bn][:, 128:256], AF.Ln, [pbt[bn], smallt], [dnt], bias=esk[:, c:c + 1])
            ACT(dn, dn, AF.Exp, [dnt], [dnt], scale=-1.0)
            yield
            TT("dve", mixT[:, c, n * 128:(n + 1) * 128], pb[bn][:, 0:128], dn, ALU.mult, [pbt[bn], dnt], [mixt[c][n]])
        pipeline(128, lambda k_: list(c1(k_)), lambda k_: list(c2(k_)))
        for t_ in (kT[0][1], kT[1][1], vtkt, cost):
            for c_ in range(8, 12):
                for m_ in mixt[c_]:
                    m_.r.extend(([t_.w] if t_.w else []) + t_.r)
        wt, wtt = w_next(8, 512)
        sc.release(0)
        xa_attend(sc, wt, wtt)
        sc.close()

    def mixer_conv(l):
        sc = Scr()
        UW = 30 + T
        uT = [sc.get([UW], BF16) for _ in range(2)]
        dgs = [sc.get([31, 128], BF16) for _ in range(2)]
        sg = [sc.get([GS], BF16) for _ in range(2)]
        cwv = vec[:, vidx["cw"]:vidx["cw"] + 8 * 31].rearrange("p (c w) -> p c w", c=8)
        for u_, ut_ in uT:
            P.op("pool", lambda e, u_=u_: e.memset(u_[:, 0:30], 0.0), [], [ut_])
        su = 0
        wts = {}

        def build_dg(c):
            dg, dgt = dgs[c % 2]
            for w_ in range(31):
                ACT(dg[:, w_, :], idb, AF.Copy, [cstbt, vect], [dgt], scale=cwv[:, c, w_:w_ + 1])

        def glu(c):
            blk, j = divmod(c, 2)
            if blk not in wts:
                wts[blk] = w_next(8, 512)
            wt, wtt = wts[blk]
            u_, ut_ = uT[c % 2]
            for g in range(NG):
                ba = proj_fm(wt, wtt, j * 128, g)
                bg = proj_fm(wt, wtt, 256 + j * 128, g)
                s_, st_ = sg[(c * NG + g) % 2]
                ACT(s_, pb[bg][:], AF.Sigmoid, [pbt[bg]], [st_])
                TT("dve", u_[:, 30 + g * GS:30 + (g + 1) * GS], pb[ba][:], s_, ALU.mult, [pbt[ba], st_], [ut_])

        def conv(c):
            u_, ut_ = uT[c % 2]
            dg, dgt = dgs[c % 2]
            for g in range(NG):
                b = nb()
                for w_ in range(31):
                    MM(pb[b][:], dg[:, w_, :], u_[:, g * GS + w_:g * GS + w_ + GS], w_ == 0, w_ == 30,
                       [dgt, ut_], [pbt[b]])
                ACT(mixT[:, c, gs(g)], pb[b][:], AF.Identity, [pbt[b], vect], mt_grp(c, g), bias=V("cb", c))

        for c in range(9):
            if c < 8:
                build_dg(c)
                glu(c)
            if c >= 1:
                conv(c - 1)
        sc.release(0)
        ysqs = [sc.get([GS], BF16) for _ in range(2)]
        mus = [sc.get([GS], F32) for _ in range(2)]
        m2s = [sc.get([GS], F32) for _ in range(2)]
        rss = [sc.get([GS], F32) for _ in range(2)]
        tt_ = [sc.get([GS], F32) for _ in range(2)]
        st = {}

        def l1(g):
            ysq, ysqt = ysqs[g % 2]
            b1 = nb()
            for c in range(8):
                MM(pb[b1][:], onesb, mixT[:, c, gs(g)], c == 0, c == 7, [cstbt] + mt_grp(c, g), [pbt[b1]])
            b2 = nb()
            for c in range(8):
                ACT(ysq, mixT[:, c, gs(g)], AF.Square, mt_grp(c, g), [ysqt])
                MM(pb[b2][:], onesb, ysq, c == 0, c == 7, [cstbt, ysqt], [pbt[b2]], inc=True)
            st[g] = (b1, b2)

        def l2(g):
            b1, b2 = st[g]
            mu, mut = mus[g % 2]
            m2, m2t = m2s[g % 2]
            rs, rst = rss[g % 2]
            ACT(mu, pb[b1][:], AF.Copy, [pbt[b1]], [mut], scale=1.0 / D)
            TT("pool", m2, mu, mu, ALU.mult, [mut], [m2t])
            STT(m2, pb[b2][:], 1.0 / D, m2, ALU.mult, ALU.subtract, [pbt[b2], m2t], [m2t])
            ACT(rs, m2, AF.Ln, [m2t, cstt], [rst], bias=CC(C_EPSL), scale=1.0)
            ACT(rs, rs, AF.Exp, [rst], [rst], scale=-0.5)
            for c in range(8):
                t_, ttt = tt_[c % 2]
                TT("dve", t_, mixT[:, c, gs(g)], mu, ALU.subtract, mt_grp(c, g) + [mut], [ttt])
                TT("dve", t_, t_, rs, ALU.mult, [ttt, rst], [ttt])
                ACT(mixT[:, c, gs(g)], t_, AF.Silu, [ttt, vect], mt_grp(c, g), bias=V("lnb", c), scale=V("lng", c))
        pipeline(NG, l1, l2)
        wt, wtt = w_next(8, 512)
        sc.release(0)
        xa_attend(sc, wt, wtt)
        sc.close()

    for li, l in enumerate(layers):
        if li == 0:
            sc0 = Scr()
            xb0 = xa_prep_p1(l, sc0)
            for _ in xa_prep_p2a(l, xb0):
                pass
            xa_prep_p2(l, xb0)
            xa_prep_p2v(l, xb0)
            sc0.close()
        prenorm("nmix%d" % l)
        kind = KINDS[l]
        if kind == 0:
            mixer_hgrn(l)
        elif kind == 1:
            mixer_swa(l)
        else:
            mixer_conv(l)
        out_proj(l)
        mlp(l, layers[li + 1] if li + 1 < len(layers) else None)

    sc = Scr()
    yo = [sc.get([D], F32) for _ in range(2)]
    out_toks = []
    for i in range(16):
        y_, yt_ = yo[i % 2]
        g = i // 4
        for half in range(2):
            b = nb()
            for c4 in range(4):
                c = half * 4 + c4
                TRN(pb[b][:, c4 * 128:(c4 + 1) * 128], xT[:, c, i * 128:(i + 1) * 128], idf, [xTt[c][g], cstt], [pbt[b]],
                    inc=(c4 == 3))
            CP("dve" if half == 0 else "act", y_[:, half * 512:(half + 1) * 512], pb[b][:], [pbt[b]], [yt_])
        out_toks.append(P.dma("sp", y_d[i * 128:(i + 1) * 128, :], y_, reads=[yt_]))
    sc.close()
    P.wait_tok("sp", out_toks)
    P.emit()
    return nc


_CACHE = {}


def run_layers(inp, x, layers):
    vp = pack_vecs(inp)
    vecs = vp.build()
    key = tuple(layers)
    if key not in _CACHE:
        _CACHE[key] = build_program(list(layers), vp.idx, vecs.shape[1])
    nc = _CACHE[key]
    consts = make_consts()
    wts = {}
    for l in layers:
        wts.update(layer_weights(inp, l))
    mem = np.asarray(inp["mem"], np.float32)
    pos = np.asarray(inp["positions"], np.int32)
    in_maps = []
    for b in range(8):
        m = {"x": np.ascontiguousarray(x[b]), "mem": np.ascontiguousarray(mem[b]),
             "pos": np.ascontiguousarray(pos[b:b + 1]), "consts": consts, "vecs": vecs}
        m.update(wts)
        in_maps.append(m)
    res = run_bass_kernel_spmd(nc, in_maps, core_ids=list(range(8)))
    return np.stack([np.asarray(r["y"], np.float32) for r in res.results], axis=0)


LAUNCH_PLAN = [[0, 1, 2, 3]]


def kernel(**inputs):
    x = np.asarray(inputs["x"], np.float32)
    for group in LAUNCH_PLAN:
        x = run_layers(inputs, x, group)
    return x
```
